# Optimizing a Trainium2 kernel written in Bass

```python
import jax, jax.numpy as jnp
from jax import lax
import numpy as np

D_MODEL = 2048
BATCH = 4
SEQ = 8192
DEPTH = 1

D_MIX = D_MODEL
D_GMLP = D_MIX // 2
D_ATTN = D_MIX - D_GMLP
GMLP_HEADS = 8
GMLP_HEAD_DIM = D_GMLP // GMLP_HEADS
GMLP_CHUNK = 128
ATTN_HEADS = 8
ATTN_HEAD_DIM = D_ATTN // ATTN_HEADS
DILATION_PATTERNS = ((128, 1), (512, 4), (2048, 16))
ATTN_BLOCK = 128
D_IN = 2 * D_GMLP + 3 * D_ATTN
N_GROUPS = 4
EXPERTS_PER_GROUP = 8
N_EXPERTS = N_GROUPS * EXPERTS_PER_GROUP
TOP_K = 2
D_EXPERT = D_MODEL // 2
MOE_BLOCK = 128
N_MOD = 6
EPS = 1e-6

kernel_name = "hybrid_gmlp_dilated_attn_hmoe"


def rms_norm(x, g):
    xf = x.astype(jnp.float32)
    y = xf * lax.rsqrt(jnp.mean(xf * xf, axis=-1, keepdims=True) + EPS)
    return (y * g.astype(jnp.float32)).astype(x.dtype)


def layer_norm(x, g):
    xf = x.astype(jnp.float32)
    xc = xf - jnp.mean(xf, axis=-1, keepdims=True)
    y = xc * lax.rsqrt(jnp.mean(xc * xc, axis=-1, keepdims=True) + EPS)
    return (y * g.astype(jnp.float32)).astype(x.dtype)


def gmlp_spatial_gating(u, v, w_s, b_s):
    B, S, _ = u.shape
    n_chunks = S // GMLP_CHUNK
    vc = v.reshape(B, n_chunks, GMLP_CHUNK, GMLP_HEADS, GMLP_HEAD_DIM)
    causal = jnp.tril(jnp.ones((GMLP_CHUNK, GMLP_CHUNK), dtype=bool))
    wm = jnp.where(causal[None], w_s, jnp.zeros_like(w_s)).astype(v.dtype)
    sv = jnp.einsum('hts,bcshd->bcthd', wm, vc) + b_s.T[:, :, None].astype(v.dtype)
    return u * sv.reshape(B, S, D_GMLP)


def dilated_window_attention(q, k, v, window, dilation):
    B, S, H, E = q.shape
    band = window // dilation
    L = S // dilation
    nb = -(-L // ATTN_BLOCK)
    Lp = nb * ATTN_BLOCK
    blk = ATTN_BLOCK

    def to_blocks(t):
        t = t.reshape(B, L, dilation, H, E)
        t = jnp.pad(t, ((0, 0), (0, Lp - L), (0, 0), (0, 0), (0, 0)))
        return t.reshape(B, nb, blk, dilation, H, E)

    qb, kb, vb = to_blocks(q), to_blocks(k), to_blocks(v)

    def with_prev(t):
        prev = jnp.concatenate([jnp.zeros_like(t[:, :1]), t[:, :-1]], axis=1)
        return jnp.concatenate([prev, t], axis=2)

    kk, vv = with_prev(kb), with_prev(vb)
    s = jnp.einsum('bnqrhe,bnkrhe->bnrhqk', qb, kk,
                   preferred_element_type=jnp.float32) * (E ** -0.5)
    qi = jnp.arange(blk)[:, None]
    kj = jnp.arange(2 * blk)[None, :]
    dist = blk + qi - kj
    in_band = (dist >= 0) & (dist <= band)
    has_prev = (jnp.arange(nb)[:, None, None] > 0) | (kj[None] >= blk)
    mask = (in_band[None] & has_prev)[None, :, None, None]
    s = jnp.where(mask, s, -jnp.inf)
    m = jnp.max(s, axis=-1, keepdims=True)
    p = jnp.exp(s - m)
    l = jnp.sum(p, axis=-1)
    o = jnp.einsum('bnrhqk,bnkrhe->bnqrhe', p, vv.astype(jnp.float32))
    l_t = jnp.transpose(l, (0, 1, 4, 2, 3))
    lse_t = jnp.transpose(m[..., 0], (0, 1, 4, 2, 3)) + jnp.log(l_t)
    o = o / l_t[..., None]
    o = o.reshape(B, Lp, dilation, H, E)[:, :L].reshape(B, S, H, E)
    lse = lse_t.reshape(B, Lp, dilation, H)[:, :L].reshape(B, S, H)
    return o, lse


def dilated_mixture_attention(q, k, v):
    outs, lses = [], []
    for window, dilation in DILATION_PATTERNS:
        o, lse = dilated_window_attention(q, k, v, window, dilation)
        outs.append(o)
        lses.append(lse)
    w = jax.nn.softmax(jnp.stack(lses, axis=0), axis=0)
    o = jnp.sum(w[..., None] * jnp.stack(outs, axis=0), axis=0)
    return o.astype(q.dtype)


def hierarchical_moe(h, w_rg, b_rg, w_re, b_re, w_gate, w_up, w_down):
    B, S, D = h.shape
    N = B * S
    hf = h.reshape(N, D)
    pg = jax.nn.softmax((hf @ w_rg + b_rg).astype(jnp.float32), axis=-1)
    gp, gi = lax.top_k(pg, 1)
    le = (jnp.einsum('nd,gde->nge', hf, w_re) + b_re).astype(jnp.float32)
    le = jnp.take_along_axis(le, gi[:, :, None], axis=1)[:, 0]
    pe = jax.nn.softmax(le, axis=-1)
    ep, ei = lax.top_k(pe, TOP_K)
    ep = ep / jnp.sum(ep, axis=-1, keepdims=True)
    weights = gp * ep
    expert_id = gi * EXPERTS_PER_GROUP + ei

    A = N * TOP_K
    e_flat = expert_id.reshape(A).astype(jnp.int32)
    tok = jnp.repeat(jnp.arange(N, dtype=jnp.int32), TOP_K)
    wt = weights.reshape(A)
    order = jnp.argsort(e_flat)
    sorted_e = e_flat[order]
    counts = jnp.bincount(e_flat, length=N_EXPERTS).astype(jnp.int32)
    padded = ((counts + MOE_BLOCK - 1) // MOE_BLOCK) * MOE_BLOCK
    pend = jnp.cumsum(padded)
    pstart = pend - padded
    cstart = jnp.cumsum(counts) - counts
    rank = jnp.arange(A, dtype=jnp.int32) - cstart[sorted_e]
    dest = pstart[sorted_e] + rank
    n_blocks = (A + N_EXPERTS * (MOE_BLOCK - 1) + MOE_BLOCK - 1) // MOE_BLOCK
    P = n_blocks * MOE_BLOCK
    row_tok = jnp.zeros((P,), jnp.int32).at[dest].set(tok[order])
    row_w = jnp.zeros((P,), jnp.float32).at[dest].set(wt[order])
    block_starts = jnp.arange(n_blocks, dtype=jnp.int32) * MOE_BLOCK
    block_e = jnp.minimum(jnp.searchsorted(pend, block_starts, side='right'),
                          N_EXPERTS - 1).astype(jnp.int32)
    xs = hf[row_tok].reshape(n_blocks, MOE_BLOCK, D)

    def expert_block(args):
        xb, e = args
        return (jax.nn.silu(xb @ w_gate[e]) * (xb @ w_up[e])) @ w_down[e]

    ys = lax.map(expert_block, (xs, block_e)).reshape(P, D)
    ys = ys * row_w[:, None].astype(ys.dtype)
    out = jax.ops.segment_sum(ys, row_tok, num_segments=N)
    return out.reshape(B, S, D).astype(h.dtype)


def setup_inputs(seed: int = 0) -> dict:
    key = jax.random.key(seed)
    ks = jax.random.split(key, 24)
    f32 = jnp.float32
    n = lambda k, shape, s: (jax.random.normal(k, shape, f32) * s)
    gain = lambda k, shape: 1.0 + 0.05 * jax.random.normal(k, shape, f32)
    L = DEPTH
    return {
        "x": n(ks[0], (BATCH, SEQ, D_MODEL), 1.0),
        "c": n(ks[1], (BATCH, D_MODEL), 1.0),
        "w_mod": n(ks[2], (L, D_MODEL, N_MOD * D_MODEL), 0.2 * D_MODEL ** -0.5),
        "b_mod": n(ks[3], (L, N_MOD * D_MODEL), 0.01),
        "g_pre_mix": gain(ks[4], (L, D_MODEL)),
        "g_post_mix": gain(ks[5], (L, D_MODEL)),
        "w_in": n(ks[6], (L, D_MODEL, D_IN), D_MODEL ** -0.5),
        "g_gmlp_v": gain(ks[7], (L, D_GMLP)),
        "w_spatial": n(ks[8], (L, GMLP_HEADS, GMLP_CHUNK, GMLP_CHUNK), 0.5 * GMLP_CHUNK ** -0.5),
        "b_spatial": gain(ks[9], (L, GMLP_HEADS, GMLP_CHUNK)),
        "g_out_gmlp": gain(ks[10], (L, D_GMLP)),
        "g_out_attn": gain(ks[11], (L, D_ATTN)),
        "w_out": n(ks[12], (L, D_MIX, D_MODEL), D_MIX ** -0.5),
        "g_pre_ffn": gain(ks[13], (L, D_MODEL)),
        "g_post_ffn": gain(ks[14], (L, D_MODEL)),
        "w_router_group": n(ks[15], (L, D_MODEL, N_GROUPS), D_MODEL ** -0.5),
        "b_router_group": n(ks[16], (L, N_GROUPS), 0.01),
        "w_router_expert": n(ks[17], (L, N_GROUPS, D_MODEL, EXPERTS_PER_GROUP), D_MODEL ** -0.5),
        "b_router_expert": n(ks[18], (L, N_GROUPS, EXPERTS_PER_GROUP), 0.01),
        "w_gate": n(ks[19], (L, N_EXPERTS, D_MODEL, D_EXPERT), D_MODEL ** -0.5),
        "w_up": n(ks[20], (L, N_EXPERTS, D_MODEL, D_EXPERT), D_MODEL ** -0.5),
        "w_down": n(ks[21], (L, N_EXPERTS, D_EXPERT, D_MODEL), D_EXPERT ** -0.5),
    }


def reference(x, c, w_mod, b_mod, g_pre_mix, g_post_mix, w_in, g_gmlp_v, w_spatial,
              b_spatial, g_out_gmlp, g_out_attn, w_out, g_pre_ffn, g_post_ffn,
              w_router_group, b_router_group, w_router_expert, b_router_expert,
              w_gate, w_up, w_down):
    B, S, D = x.shape
    for l in range(DEPTH):
        mod = jax.nn.silu(c) @ w_mod[l] + b_mod[l]
        shift1, scale1, gate1, shift2, scale2, gate2 = [
            t[:, None, :] for t in jnp.split(mod, N_MOD, axis=-1)]

        h = rms_norm(x, g_pre_mix[l]) * (1 + scale1) + shift1
        proj = h @ w_in[l]
        u, v, q, k, va = jnp.split(
            proj, [D_GMLP, 2 * D_GMLP, 2 * D_GMLP + D_ATTN, 2 * D_GMLP + 2 * D_ATTN], axis=-1)
        u = jax.nn.gelu(u)
        v = layer_norm(jax.nn.gelu(v), g_gmlp_v[l])
        ya = gmlp_spatial_gating(u, v, w_spatial[l], b_spatial[l])
        hs = (B, S, ATTN_HEADS, ATTN_HEAD_DIM)
        yb = dilated_mixture_attention(q.reshape(hs), k.reshape(hs), va.reshape(hs))
        yb = yb.reshape(B, S, D_ATTN)
        y = jnp.concatenate([rms_norm(ya, g_out_gmlp[l]), rms_norm(yb, g_out_attn[l])], axis=-1)
        y = y @ w_out[l]
        x = x + gate1 * rms_norm(y, g_post_mix[l])

        h = rms_norm(x, g_pre_ffn[l]) * (1 + scale2) + shift2
        y = hierarchical_moe(h, w_router_group[l], b_router_group[l], w_router_expert[l],
                             b_router_expert[l], w_gate[l], w_up[l], w_down[l])
        x = x + gate2 * rms_norm(y, g_post_ffn[l])
    return x
```

```python
import os
from contextlib import ExitStack

import numpy as np
import concourse.bass as bass
import concourse.mybir as mybir
from concourse.bass_utils import run_bass_kernel_spmd

F32, BF16, I32 = mybir.dt.float32, mybir.dt.bfloat16, mybir.dt.int32
AF = mybir.ActivationFunctionType
ALU = mybir.AluOpType
AX = mybir.AxisListType

P = 128
D = 2048
KC = 16
TOWN = 4096
THALO = 2048
TEXT = TOWN + THALO
DG = 1024
NH = 8
DIN = 5120
NE = 32
DE = 1024
CAP = 512
NSLOT = NE * CAP
EPS = 1e-6
NEG = -30000.0
NT = TOWN // P

STOP_AFTER = os.environ.get("MK_STOP_AFTER", "")
DEBUG = bool(STOP_AFTER)


class _DummyIns:
    def then_inc(self, *a, **k):
        return self


class _DummyEngine:
    def __getattr__(self, name):
        def f(*a, **k):
            return _DummyIns()
        return f


_DUMMY = _DummyEngine()


class Eng:
    def __init__(self, b, e, name):
        self.b, self.real, self.name = b, e, name
        self.sem = b.newsem(name + "_prog")
        self.n = 0
        self.seen = {}
        self.muted = False

    @property
    def e(self):
        return _DUMMY if self.muted else self.real

    def wait(self, *toks):
        for t in toks:
            if t is None:
                continue
            if isinstance(t, list):
                self.wait(*t)
                continue
            sem, val = t
            if self.seen.get(sem.num, 0) < val:
                if not self.muted:
                    self.real.wait_ge(sem, val)
                self.seen[sem.num] = val

    def mark(self, ins):
        self.n += 1
        ins.then_inc(self.sem, 1)
        return (self.sem, self.n)

    def last(self):
        return (self.sem, self.n) if self.n else None


class Slot:
    def __init__(self, b, name):
        self.sem = b.newsem(name)
        self.n = 0
        b.slots.append(self)

    def dma(self, ins):
        self.n += 16
        ins.then_inc(self.sem, 16)
        return (self.sem, self.n)

    def last(self):
        return (self.sem, self.n) if self.n else None


class Builder:
    def __init__(self):
        self.nc = bass.Bass("TRN2", target_bir_lowering=False)
        self.es = ExitStack()
        self.slots = []
        self._semn = 0
        nc = self.nc
        self.pe = Eng(self, nc.tensor, "pe")
        self.act = Eng(self, nc.scalar, "act")
        self.dve = Eng(self, nc.vector, "dve")
        self.pool = Eng(self, nc.gpsimd, "pool")
        self.sp = Eng(self, nc.sync, "sp")
        self.engs = [self.pe, self.act, self.dve, self.pool, self.sp]

    def newsem(self, name):
        self._semn += 1
        return self.es.enter_context(self.nc.semaphore(f"{name}_{self._semn}"))

    def barrier(self):
        toks = [e.last() for e in self.engs] + [s.last() for s in self.slots]
        for e in self.engs:
            e.wait(*toks)


def build():
    b = Builder()
    nc = b.nc
    pe, act, dve, pool, sp = b.pe, b.act, b.dve, b.pool, b.sp

    def din(name, shape, dt=F32):
        return nc.dram_tensor(name, list(shape), dt, kind="ExternalInput").ap()

    def dscr(name, shape, dt, dbg=False):
        kind = "ExternalOutput" if (DEBUG and dbg) else "Internal"
        return nc.dram_tensor(name, list(shape), dt, kind=kind).ap()

    x_d = din("x", [TOWN, D])
    xh_d = din("xh", [THALO, D])
    c_d = din("c", [P, KC])
    wmod_d = din("w_mod", [D, 6 * D])
    bmod_d = din("b_mod", [1, 6 * D])
    gpre1_d = din("g_pre_mix", [1, D])
    gpost1_d = din("g_post_mix", [1, D])
    gpre2_d = din("g_pre_ffn", [1, D])
    gpost2_d = din("g_post_ffn", [1, D])
    ggv_d = din("g_gmlp_v", [1, DG])
    gout_d = din("g_out", [P, KC])
    win_d = din("w_in", [D, DIN])
    wout_d = din("w_out", [D, D])
    wsp_d = din("w_sp", [NH, P, P])
    bsp_d = din("b_sp", [1, NH * P])
    wr_d = din("w_r", [D, 36])
    br_d = din("b_r", [1, 36])
    wg_d = din("w_gate", [NE, D, DE])
    wu_d = din("w_up", [NE, D, DE])
    wd_d = din("w_down", [NE, DE, D])
    ident_d = din("ident", [P, P])
    tril_d = din("tril", [P, P])
    ustr_d = din("ustrict", [P, P])
    maskb_d = din("maskb", [P, 3, P])
    ecap_d = din("ecap", [1, NE])
    out_d = nc.dram_tensor("out", [TOWN, D], F32, kind="ExternalOutput").ap()

    mod_s = dscr("mod_s", [1, 6 * D], F32, True)
    uT_s = dscr("uT_s", [NH, P, TOWN], BF16, True)
    gv_s = dscr("gv_s", [TOWN, DG], BF16, True)
    qT_s = dscr("qT_s", [NH, P, TOWN], BF16, True)
    kT_s = dscr("kT_s", [NH, P, TEXT], BF16, True)
    v_s = dscr("v_s", [TEXT, DG], BF16, True)
    yT_s = dscr("yT_s", [KC, P, TOWN], BF16, True)
    x1_s = dscr("x1_s", [TOWN, D], F32, True)
    xe_s = dscr("xe_s", [NSLOT + P, D], BF16, False)
    ye_s = dscr("ye_s", [NSLOT, D], BF16, False)
    rt_s = dscr("rt_s", [P, NT * 4], F32, True)

    es = b.es

    def sb(name, shape, dt, stack=None):
        return (stack or es).enter_context(nc.sbuf_tensor("sb_" + name, list(shape), dt))

    def ps(name, shape, dt, stack=None):
        return (stack or es).enter_context(nc.psum_tensor("ps_" + name, list(shape), dt))

    ident_f = sb("ident_f", [P, P], F32)
    ident_b = sb("ident_b", [P, P], BF16)
    ones_b = sb("ones_b", [P, P], BF16)
    neghalf = sb("neghalf", [P, 512], F32)
    eps_t = sb("eps_t", [P, 1], F32)
    lnst = sb("lnst", [P, NT, 4], F32)
    ridx = sb("ridx", [P, NT, 2], I32)
    rwt = sb("rwt", [P, NT, 2], F32)
    cnt_i = sb("cnt_i", [P, NE], I32)

    ld0 = Slot(b, "ld0")
    t_id = ld0.dma(sp.e.dma_start(out=ident_f[:], in_=ident_d))
    dve.wait(t_id)
    dve.mark(dve.e.tensor_copy(out=ident_b[:], in_=ident_f[:]))
    dve.mark(dve.e.memset(ones_b[:], 1.0))
    dve.mark(dve.e.memset(neghalf[:], -0.5))
    dve.mark(dve.e.memset(lnst[:], 0.0))
    dve.mark(dve.e.memset(eps_t[:], EPS))
    t_const = dve.last()

    def rsqrt_small(ss_ap, ms_ap, out_ap, scale, n, after):
        dve.wait(after)
        t = dve.mark(dve.e.tensor_scalar(out=ms_ap, in0=ss_ap, scalar1=scale, scalar2=EPS,
                                         op0=ALU.mult, op1=ALU.add))
        pool.wait(t, t_const)
        return pool.mark(pool.e.tensor_tensor(out=out_ap, in0=ms_ap, in1=neghalf[:, 0:n], op=ALU.pow))

    sc = sb("sc", [P, KC], BF16)
    NPC = 6 * D // 512
    NPC0 = 8

    class ModCalc:
        def __init__(self, ph, tag):
            self.NB = 3
            self.wm = [sb(f"wm{tag}{i}", [P, KC, 512], BF16, ph) for i in range(self.NB)]
            self.bp = [sb(f"bp{tag}{i}", [1, 512], F32, ph) for i in range(self.NB)]
            self.mp = [sb(f"mp{tag}{i}", [1, 512], F32, ph) for i in range(2)]
            self.mps = [ps(f"mps{tag}{i}", [1, 512], F32, ph) for i in range(2)]
            self.ws = [Slot(b, f"wm{tag}_s{i}") for i in range(self.NB)]
            self.bs = [Slot(b, f"bp{tag}_s{i}") for i in range(self.NB)]
            self.ms = [Slot(b, f"mp{tag}_s{i}") for i in range(2)]
            self.wfree = [None] * self.NB
            self.bfree = [None] * self.NB
            self.mfree = [None, None]
            self.psfree = [None, None]
            self.tok = {}
            self.ni = 0
            self.nc_ = 0

        def issue(self, j):
            s = self.ni % self.NB
            s2 = self.ni % 2
            self.ni += 1
            pool.wait(self.wfree[s])
            tw = self.ws[s].dma(pool.e.dma_start(
                out=self.wm[s][:], in_=wmod_d[:, j * 512:(j + 1) * 512].rearrange("(k p) f -> p k f", p=P)))
            sp.wait(self.bfree[s])
            tb = self.bs[s].dma(sp.e.dma_start(out=self.bp[s][:], in_=bmod_d[0:1, j * 512:(j + 1) * 512]))
            self.tok[j] = (s, s2, tw, tb)

        def compute(self, j):
            s, s2, tw, tb = self.tok[j]
            pe.wait(tw, t_sc, self.psfree[s2])
            for k in range(KC):
                ins = pe.e.matmul(self.mps[s2][:], lhsT=sc[:, k:k + 1], rhs=self.wm[s][:, k, :],
                                  start=(k == 0), stop=(k == KC - 1))
            t_pe = pe.mark(ins)
            self.wfree[s] = t_pe
            dve.wait(t_pe, tb, self.mfree[s2])
            t_ev = dve.mark(dve.e.tensor_tensor(out=self.mp[s2][:], in0=self.mps[s2][:], in1=self.bp[s][:], op=ALU.add))
            self.psfree[s2] = t_ev
            self.bfree[s] = t_ev
            sp.wait(t_ev)
            self.mfree[s2] = self.ms[s2].dma(sp.e.dma_start(out=mod_s[0:1, j * 512:(j + 1) * 512], in_=self.mp[s2][:]))

    with ExitStack() as ph:
        c_t = sb("c_t", [P, KC], F32, ph)
        t_c = ld0.dma(sp.e.dma_start(out=c_t[:], in_=c_d))
        act.wait(t_c)
        t_sc = act.mark(act.e.activation(out=sc[:], in_=c_t[:], func=AF.Silu))
        mc = ModCalc(ph, "0")
        for j in range(3):
            mc.issue(j)
        for j in range(NPC0):
            mc.compute(j)
            if j + 3 < NPC0:
                mc.issue(j + 3)
        b.barrier()
    if STOP_AFTER == "0":
        return b, finish(b, out_d)

    with ExitStack() as ph:
        gm_bc = sb("gm1_bc", [P, D], F32, ph)
        sh_bc = sb("sh1_bc", [P, D], F32, ph)
        hT = sb("hT", [P, KC, 2048], BF16, ph)
        xt = [sb(f"xtA{i}", [P, D], F32, ph) for i in range(2)]
        t1 = sb("t1A", [P, D], F32, ph)
        xn = [sb(f"xnA{i}", [P, D], BF16, ph) for i in range(2)]
        junk = sb("junkA", [P, D], BF16, ph)
        ssA = sb("ssA", [P, 48], F32, ph)
        msA = sb("msA", [P, 48], F32, ph)
        rsA = sb("rsA", [P, 48], F32, ph)
        NBW = 3
        wr_ = [sb(f"wA{i}", [P, KC, 512], BF16, ph) for i in range(NBW)]
        NST = 4
        stg = [sb(f"stgA{i}", [P, 512], BF16, ph) for i in range(NST)]
        sqj = sb("sqjA", [P, 512], BF16, ph)
        tpp = [ps(f"tpA{i}", [P, 4, P], BF16, ph) for i in range(2)]
        mmp = [ps(f"mmA{i}", [P, 512], F32, ph) for i in range(4)]

        xs = [Slot(b, f"xA_s{i}") for i in range(2)]
        ws = [Slot(b, f"wA_s{i}") for i in range(NBW)]
        sts = [Slot(b, f"stA_s{i}") for i in range(NST)]
        cs = Slot(b, "constA")

        t_a = cs.dma(sp.e.dma_start(out=gm_bc[:], in_=gpre1_d.to_broadcast([P, D])))
        t_b2 = cs.dma(sp.e.dma_start(out=t1[:], in_=mod_s[0:1, D:2 * D].to_broadcast([P, D])))
        t_c2 = cs.dma(sp.e.dma_start(out=sh_bc[:], in_=mod_s[0:1, 0:D].to_broadcast([P, D])))
        dve.wait(t_a, t_b2, t_c2)
        t_gm = dve.mark(dve.e.scalar_tensor_tensor(out=gm_bc[:], in0=t1[:], scalar=1.0, in1=gm_bc[:],
                                                   op0=ALU.add, op1=ALU.mult))
        t_ssz = dve.mark(dve.e.memset(ssA[:], 0.0))

        spans = [("halo", xh_d, 0, [6, 7, 8, 9]), ("own0", x_d, 0, list(range(10))),
                 ("own1", x_d, 2048, list(range(10)))]
        gtile = 0
        xfree = [None, None]
        t1free = t_gm
        xnfree = [None, None]
        tpfree = [None, None]
        mmfree = [None] * 4
        stfree = [None] * NST
        wfree = [None] * NBW
        mmi = 0
        sti = 0
        wi = 0
        hT_readers = None

        for (sname, xsrc, xoff, blocks) in spans:
            is_halo = sname == "halo"
            ext_off = 0 if is_halo else (2048 + xoff)
            w_tok = {}
            pend = list(blocks)

            def issue_wA(blk):
                nonlocal wi
                s = wi % NBW
                wi += 1
                pool.wait(wfree[s])
                w_tok[blk] = (s, ws[s].dma(pool.e.dma_start(
                    out=wr_[s][:], in_=win_d[:, blk * 512:(blk + 1) * 512].rearrange("(k p) f -> p k f", p=P))))

            for _ in range(min(NBW, len(pend))):
                issue_wA(pend.pop(0))

            hT_ready = []
            xn_tok = {}

            def stage_X(i):
                nonlocal gtile, t1free
                s2 = gtile % 2
                row0 = xoff + i * P
                sp.wait(xfree[s2])
                t_x = xs[s2].dma(sp.e.dma_start(out=xt[s2][:], in_=xsrc[row0:row0 + P, :]))
                act.wait(t_x, t_ssz)
                t_ss = act.mark(act.e.activation(out=junk[:], in_=xt[s2][:], func=AF.Square,
                                                 accum_out=ssA[:, gtile:gtile + 1]))
                t_rs = rsqrt_small(ssA[:, gtile:gtile + 1], msA[:, gtile:gtile + 1], rsA[:, gtile:gtile + 1],
                                   1.0 / D, 1, t_ss)
                dve.wait(t_rs, t_x, t1free, t_gm)
                t_t1 = dve.mark(dve.e.scalar_tensor_tensor(out=t1[:], in0=xt[s2][:], scalar=rsA[:, gtile:gtile + 1],
                                                           in1=gm_bc[:], op0=ALU.mult, op1=ALU.mult))
                xfree[s2] = t_t1
                pool.wait(t_t1, xnfree[s2], t_c2)
                t_xn = pool.mark(pool.e.tensor_tensor(out=xn[s2][:], in0=t1[:], in1=sh_bc[:], op=ALU.add))
                t1free = t_xn
                xn_tok[i] = (s2, t_xn)
                gtile += 1

            def stage_Y(i):
                s2, t_xn = xn_tok[i]
                for g in range(4):
                    pb = g % 2
                    pe.wait(t_xn, tpfree[pb])
                    if i == 0:
                        pe.wait(hT_readers)
                    for j in range(4):
                        k = 4 * g + j
                        ins = pe.e.transpose(out=tpp[pb][:, j, :], in_=xn[s2][:, k * P:(k + 1) * P],
                                             identity=ident_b[:])
                    t_tp = pe.mark(ins)
                    ev = act if g % 2 == 0 else dve
                    ev.wait(t_tp)
                    if i == 0:
                        ev.wait(hT_readers)
                    if ev is act:
                        t_ev = act.mark(act.e.copy(out=hT[:, 4 * g:4 * g + 4, i * P:(i + 1) * P], in_=tpp[pb][:]))
                    else:
                        t_ev = dve.mark(dve.e.tensor_copy(out=hT[:, 4 * g:4 * g + 4, i * P:(i + 1) * P],
                                                          in_=tpp[pb][:]))
                    tpfree[pb] = t_ev
                    hT_ready.append(t_ev)
                xnfree[s2] = t_tp

            stage_X(0)
            for i in range(16):
                if i + 1 < 16:
                    stage_X(i + 1)
                stage_Y(i)

            def stage_out(src_ps, kind, dst_ap, pe_tok, extra=None):
                nonlocal sti
                s = sti % NST
                sti += 1
                if kind == "gelu":
                    act.wait(pe_tok, stfree[s])
                    if extra is not None:
                        t_e = act.mark(act.e.activation(out=stg[s][:], in_=src_ps[:], func=AF.Gelu,
                                                        accum_out=extra[0]))
                        dve.wait(t_e)
                        t_q = dve.mark(dve.e.tensor_tensor(out=sqj[:], in0=stg[s][:], in1=stg[s][:], op=ALU.mult))
                        dve.wait(t_q)
                        t_q2 = dve.mark(dve.e.reduce_sum(out=extra[1], in_=sqj[:], axis=AX.X))
                    else:
                        t_e = act.mark(act.e.activation(out=stg[s][:], in_=src_ps[:], func=AF.Gelu))
                else:
                    dve.wait(pe_tok, stfree[s])
                    t_e = dve.mark(dve.e.tensor_copy(out=stg[s][:], in_=src_ps[:]))
                sp.wait(t_e)
                stfree[s] = [sts[s].dma(sp.e.dma_start(out=dst_ap, in_=stg[s][:]))]
                if kind == "gelu" and extra is not None:
                    stfree[s].append(t_q)
                return t_e

            for blk in blocks:
                (s, t_w) = w_tok[blk]
                fm = blk in (0, 1, 4, 5, 6, 7)
                if fm:
                    for m in range(4):
                        head = (blk % 2) * 4 + m
                        for st in range(4):
                            pb = mmi % 4
                            mmi += 1
                            pe.wait(t_w, hT_ready, mmfree[pb])
                            for k in range(KC):
                                ins = pe.e.matmul(mmp[pb][:], lhsT=wr_[s][:, k, m * P:(m + 1) * P],
                                                  rhs=hT[:, k, st * 512:(st + 1) * 512],
                                                  start=(k == 0), stop=(k == KC - 1))
                            t_mm = pe.mark(ins)
                            if blk in (0, 1):
                                dst = uT_s[head, :, xoff + st * 512: xoff + (st + 1) * 512]
                                mmfree[pb] = stage_out(mmp[pb], "gelu", dst, t_mm)
                            elif blk in (4, 5):
                                dst = qT_s[head, :, xoff + st * 512: xoff + (st + 1) * 512]
                                mmfree[pb] = stage_out(mmp[pb], "copy", dst, t_mm)
                            else:
                                dst = kT_s[head, :, ext_off + st * 512: ext_off + (st + 1) * 512]
                                mmfree[pb] = stage_out(mmp[pb], "copy", dst, t_mm)
                else:
                    half = blk % 2
                    for i in range(16):
                        pb = mmi % 4
                        mmi += 1
                        pe.wait(t_w, hT_ready, mmfree[pb])
                        for k in range(KC):
                            ins = pe.e.matmul(mmp[pb][:], lhsT=hT[:, k, i * P:(i + 1) * P], rhs=wr_[s][:, k, :],
                                              start=(k == 0), stop=(k == KC - 1))
                        t_mm = pe.mark(ins)
                        if blk in (2, 3):
                            ot = (xoff // P) + i
                            dst = gv_s[xoff + i * P: xoff + (i + 1) * P, half * 512:(half + 1) * 512]
                            mmfree[pb] = stage_out(mmp[pb], "gelu", dst, t_mm,
                                                   extra=(lnst[:, ot, half:half + 1], lnst[:, ot, 2 + half:3 + half]))
                        else:
                            dst = v_s[ext_off + i * P: ext_off + (i + 1) * P, half * 512:(half + 1) * 512]
                            mmfree[pb] = stage_out(mmp[pb], "copy", dst, t_mm)
                wfree[s] = pe.last()
                hT_readers = pe.last()
                if pend:
                    issue_wA(pend.pop(0))
        b.barrier()
    if STOP_AFTER == "A":
        return b, finish(b, out_d)

    class HeadNorm:
        def __init__(self, ph, tag, gcol):
            self.sq = sb("sq" + tag, [P, NH, 512], BF16, ph)
            self.msb = sb("msb" + tag, [P, 512], F32, ph)
            self.rsb = sb("rsb" + tag, [P, 512], F32, ph)
            self.ynT = [sb(f"ynT{tag}{i}", [P, NH, 512], BF16, ph) for i in range(2)]
            self.ssb = ps("ssb" + tag, [P, 512], F32, ph)
            self.yns = [Slot(b, f"yn{tag}_s{i}") for i in range(2)]
            self.gcol = gcol
            self.ynfree = [None, None]
            self.ssbfree = None
            self.sqfree = None
            self.rsfree = None
            self.n = 0

        def run(self, st, src, t_src, chunk0):
            act.wait(t_src, self.sqfree)
            t_sq = act.mark(act.e.activation(out=self.sq[:], in_=src, func=AF.Square))
            pe.wait(t_sq, self.ssbfree)
            for h in range(NH):
                ins = pe.e.matmul(self.ssb[:], lhsT=ones_b[:], rhs=self.sq[:, h, :], start=(h == 0), stop=(h == NH - 1))
            t_ss = pe.mark(ins)
            self.sqfree = t_ss
            act.wait(t_ss, self.rsfree, t_const)
            t_ln = act.mark(act.e.activation(out=self.rsb[:], in_=self.ssb[:], func=AF.Ln, bias=eps_t[:], scale=1.0 / DG))
            self.ssbfree = t_ln
            act.wait(t_ln)
            t_rs = act.mark(act.e.activation(out=self.rsb[:], in_=self.rsb[:], func=AF.Exp, scale=-0.5))
            s = self.n % 2
            self.n += 1
            dve.wait(t_rs, self.ynfree[s], t_src)
            for h in range(NH):
                ins = dve.e.scalar_tensor_tensor(out=self.ynT[s][:, h, :], in0=src[:, h, :],
                                                 scalar=self.gcol[:, chunk0 + h:chunk0 + h + 1], in1=self.rsb[:],
                                                 op0=ALU.mult, op1=ALU.mult)
            t_yn = dve.mark(ins)
            self.rsfree = t_yn
            sp.wait(t_yn)
            self.ynfree[s] = self.yns[s].dma(sp.e.dma_start(
                out=yT_s[chunk0:chunk0 + NH, :, st * 512:(st + 1) * 512].rearrange("h p t -> p h t"),
                in_=self.ynT[s][:]))
            return t_yn

    NSTB = TOWN // 512
    with ExitStack() as ph:
        gcol = sb("gcol", [P, KC], F32, ph)
        cB = Slot(b, "constB")
        t_gc = cB.dma(sp.e.dma_start(out=gcol[:], in_=gout_d))
        dve.wait(t_gc)

        with ExitStack() as p1:
            hn = HeadNorm(p1, "g", gcol)
            WsT = sb("WsT", [P, NH, P], BF16, p1)
            wtmp = sb("wtmp", [P, P], F32, p1)
            wmb = sb("wmb", [P, P], BF16, p1)
            tril_t = sb("tril_t", [P, P], F32, p1)
            bsr_f = sb("bsr_f", [1, NH * P], F32, p1)
            bsr = sb("bsr", [1, NH * P], BF16, p1)
            ggv_bc = sb("ggv_bc", [P, DG], F32, p1)
            mean = sb("meanB", [P, NT], F32, p1)
            ex2 = sb("ex2B", [P, NT], F32, p1)
            rstd = sb("rstdB", [P, NT], F32, p1)
            gvt = [sb(f"gvt{i}", [P, 4, DG], BF16, p1) for i in range(2)]
            uTt = [sb(f"uTt{i}", [P, NH, 512], BF16, p1) for i in range(2)]
            vtmp = sb("vtmp", [P, DG], F32, p1)
            vn = [sb(f"vn{i}", [P, DG], BF16, p1) for i in range(2)]
            yaT = [sb(f"yaT{i}", [P, NH, 512], BF16, p1) for i in range(2)]
            svp = [ps(f"svp{i}", [P, NH, P], F32, p1) for i in range(2)]
            tpw = ps("tpw", [P, P], BF16, p1)
            gvs = [Slot(b, f"gv_s{i}") for i in range(2)]
            uts = [Slot(b, f"ut_s{i}") for i in range(2)]

            mc2 = ModCalc(p1, "2")
            for j in range(NPC0, NPC0 + 3):
                mc2.issue(j)
            zt = sb("zt", [P, 4, D], BF16, p1)
            zs = Slot(b, "zfill")
            t_z = dve.mark(dve.e.memset(zt[:], 0.0))
            t_tr = cB.dma(sp.e.dma_start(out=tril_t[:], in_=tril_d))
            t_bs = cB.dma(sp.e.dma_start(out=bsr_f[:], in_=bsp_d))
            t_gg = cB.dma(sp.e.dma_start(out=ggv_bc[:], in_=ggv_d.to_broadcast([P, DG])))
            dve.wait(t_bs)
            t_bsr = dve.mark(dve.e.tensor_copy(out=bsr[:], in_=bsr_f[:]))
            t_prev = None
            for h in range(NH):
                sp.wait(t_prev)
                t_w = cB.dma(sp.e.dma_start(out=wtmp[:], in_=wsp_d[h]))
                dve.wait(t_w, t_tr, t_prev)
                t_m = dve.mark(dve.e.tensor_tensor(out=wmb[:], in0=wtmp[:], in1=tril_t[:], op=ALU.mult))
                pe.wait(t_m, t_prev)
                t_t = pe.mark(pe.e.transpose(out=tpw[:], in_=wmb[:], identity=ident_b[:]))
                dve.wait(t_t)
                t_prev = dve.mark(dve.e.tensor_copy(out=WsT[:, h, :], in_=tpw[:]))
            t_wst = t_prev

            def dchain(ins):
                t = dve.mark(ins)
                dve.wait(t)
                return t
            dchain(dve.e.tensor_tensor(out=mean[:], in0=lnst[:, :, 0], in1=lnst[:, :, 1], op=ALU.add))
            dchain(dve.e.tensor_scalar(out=mean[:], in0=mean[:], scalar1=1.0 / DG, scalar2=None, op0=ALU.mult))
            dchain(dve.e.tensor_tensor(out=ex2[:], in0=lnst[:, :, 2], in1=lnst[:, :, 3], op=ALU.add))
            dchain(dve.e.tensor_scalar(out=ex2[:], in0=ex2[:], scalar1=1.0 / DG, scalar2=None, op0=ALU.mult))
            dchain(dve.e.tensor_tensor(out=rstd[:], in0=mean[:], in1=mean[:], op=ALU.mult))
            dchain(dve.e.tensor_tensor(out=ex2[:], in0=ex2[:], in1=rstd[:], op=ALU.subtract))
            t_var = dchain(dve.e.tensor_scalar(out=ex2[:], in0=ex2[:], scalar1=EPS, scalar2=None, op0=ALU.add))
            pool.wait(t_var)
            t_rstd = pool.mark(pool.e.tensor_tensor(out=rstd[:], in0=ex2[:], in1=neghalf[:, 0:NT], op=ALU.pow))

            gvfree = [None, None]
            utfree = [None, None]
            ld_tok = {}
            zfill_left = list(range(NE))

            def load_st(st):
                s = st % 2
                sp.wait(gvfree[s], utfree[s])
                tg = gvs[s].dma(sp.e.dma_start(
                    out=gvt[s][:], in_=gv_s[st * 512:(st + 1) * 512, :].rearrange("(j p) f -> p j f", p=P)))
                tu = uts[s].dma(sp.e.dma_start(
                    out=uTt[s][:], in_=uT_s[:, :, st * 512:(st + 1) * 512].rearrange("h p t -> p h t")))
                ld_tok[st] = (tg, tu)

            vtfree = None
            vnfree = [None, None]
            svfree = [None, None]
            yafree = [None, None]
            cnt = 0
            load_st(0)
            for st in range(NSTB):
                if st + 1 < NSTB:
                    load_st(st + 1)
                sp.wait(t_z)
                for _ in range(4):
                    e = zfill_left.pop(0)
                    zs.dma(sp.e.dma_start(out=xe_s[e * CAP:(e + 1) * CAP, :].rearrange("(b p) f -> p b f", p=P),
                                          in_=zt[:]))
                s = st % 2
                tg, tu = ld_tok[st]
                for j in range(4):
                    ot = st * 4 + j
                    c2 = cnt % 2
                    cnt += 1
                    dve.wait(tg, t_rstd, vtfree)
                    t_v1 = dve.mark(dve.e.tensor_scalar(out=vtmp[:], in0=gvt[s][:, j, :], scalar1=mean[:, ot:ot + 1],
                                                        scalar2=rstd[:, ot:ot + 1], op0=ALU.subtract, op1=ALU.mult))
                    pool.wait(t_v1, t_gg, vnfree[c2])
                    t_vn = pool.mark(pool.e.tensor_tensor(out=vn[c2][:], in0=vtmp[:], in1=ggv_bc[:], op=ALU.mult))
                    vtfree = t_vn
                    pe.wait(t_vn, t_wst, t_bsr, svfree[c2])
                    for h in range(NH):
                        pe.e.matmul(svp[c2][:, h, :], lhsT=vn[c2][:, h * P:(h + 1) * P], rhs=WsT[:, h, :],
                                    start=True, stop=False)
                        ins = pe.e.matmul(svp[c2][:, h, :], lhsT=ones_b[0:1, :], rhs=bsr[0:1, h * P:(h + 1) * P],
                                          start=False, stop=True)
                    t_sv = pe.mark(ins)
                    vnfree[c2] = t_sv
                    dve.wait(t_sv, tu)
                    if j == 0:
                        dve.wait(yafree[s])
                    for hh in range(2):
                        ins = dve.e.tensor_tensor(out=yaT[s][:, 4 * hh:4 * hh + 4, j * P:(j + 1) * P],
                                                  in0=svp[c2][:, 4 * hh:4 * hh + 4, :],
                                                  in1=uTt[s][:, 4 * hh:4 * hh + 4, j * P:(j + 1) * P], op=ALU.mult)
                    t_ya = dve.mark(ins)
                    svfree[c2] = t_ya
                gvfree[s] = t_v1
                utfree[s] = t_ya
                yafree[s] = hn.run(st, yaT[s][:], t_ya, 0)
                for jm in (NPC0 + 2 * st, NPC0 + 2 * st + 1):
                    if jm < NPC:
                        mc2.compute(jm)
                        if jm + 3 < NPC:
                            mc2.issue(jm + 3)
            b.barrier()
        if STOP_AFTER == "B1":
            return b, finish(b, out_d)

        with ExitStack() as p2:
            maskf = sb("maskf", [P, 3, P], F32, p2)
            mpair = sb("mpair", [P, 2, 2, P], BF16, p2)
            qTt = [sb(f"qTt{i}", [P, 2048], BF16, p2) for i in range(2)]
            kTt = [sb(f"kTt{i}", [P, 4096], BF16, p2) for i in range(2)]
            NV = 20
            vpc = [sb(f"vpc{i}", [P, 2, P], BF16, p2) for i in range(NV)]
            NPT = 3
            pT = [sb(f"pT{i}", [P, 2, P], BF16, p2) for i in range(NPT)]
            acc2r = [sb(f"acc2{i}", [P, 2, 2048], F32, p2) for i in range(2)]
            rec = sb("rec", [P, 2048], F32, p2)
            ost = [sb(f"ost{i}", [P, 2048], BF16, p2) for i in range(2)]
            sps = [ps(f"sps{i}", [P, 2, P], F32, p2) for i in range(NPT)]
            olp = [ps(f"olp{i}", [P, 2, P], F32, p2) for i in range(2)]
            qs = [Slot(b, f"q_s{i}") for i in range(2)]
            ks = [Slot(b, f"k_s{i}") for i in range(2)]
            vs = [Slot(b, f"v_s{i}") for i in range(NV)]
            oss = [Slot(b, f"o_s{i}") for i in range(2)]

            t_mf = cB.dma(sp.e.dma_start(out=maskf[:], in_=maskb_d))
            dve.wait(t_mf)
            dve.mark(dve.e.tensor_single_scalar(out=mpair[:, 0, 0, :], in_=maskf[:, 1, :], scalar=0.0, op=ALU.is_equal))
            dve.mark(dve.e.tensor_single_scalar(out=mpair[:, 0, 1, :], in_=maskf[:, 0, :], scalar=0.0, op=ALU.is_equal))
            dve.mark(dve.e.tensor_single_scalar(out=mpair[:, 1, 0, :], in_=maskf[:, 2, :], scalar=0.0, op=ALU.is_equal))
            t_mask = dve.mark(dve.e.tensor_single_scalar(out=mpair[:, 1, 1, :], in_=maskf[:, 0, :], scalar=0.0, op=ALU.is_equal))
            dve.wait(t_mask)

            units = [(h, spn) for h in range(NH) for spn in range(2)]
            qkfree = [None, None]
            vfree = [None] * NV
            pTfree = [None] * 3
            spsfree = [None] * 3
            olpfree = [None, None]
            ostfree = [None, None]
            accfree = [None, None]
            rec_free = None
            scale = float(P) ** -0.5
            bi = 0
            qk_tok = {}

            def load_qk(u):
                h, spn = units[u]
                s = u % 2
                sp.wait(qkfree[s])
                tq = qs[s].dma(sp.e.dma_start(out=qTt[s][:], in_=qT_s[h, :, spn * 2048:(spn + 1) * 2048]))
                tk = ks[s].dma(sp.e.dma_start(out=kTt[s][:], in_=kT_s[h, :, spn * 2048:spn * 2048 + 4096]))
                qk_tok[u] = (tq, tk)

            load_qk(0)
            for u, (h, spn) in enumerate(units):
                s = u % 2
                acc2 = acc2r[u % 2]
                acc_free = accfree[u % 2]
                tq, tk = qk_tok[u]
                blocks = []
                for d in (1, 4, 16):
                    Bt = P * d
                    for grp in range(2048 // Bt):
                        for r in range(d):
                            blocks.append((d, grp, r))
                nb = len(blocks)
                v_tok = {}
                s_tok = {}
                p_tok = {}
                copy_toks = []
                pat_done = {}

                def load_v(i):
                    d, grp, r = blocks[i]
                    Bt = P * d
                    q0 = grp * Bt + r
                    start = spn * 2048 + 2048 + q0 - Bt
                    vsl = (bi + i) % NV
                    sp.wait(vfree[vsl])
                    src = v_s[start:start + 255 * d + 1:d, h * P:(h + 1) * P].rearrange("(bb p) e -> p bb e", p=P)
                    v_tok[i] = (vsl, vs[vsl].dma(sp.e.dma_start(out=vpc[vsl][:], in_=src)))

                def emit_S(i):
                    d, grp, r = blocks[i]
                    Bt = P * d
                    q0 = grp * Bt + r
                    sb_ = (bi + i) % NPT
                    qsl = slice(q0, q0 + (P - 1) * d + 1, d)
                    kc0 = 2048 + q0
                    kp0 = kc0 - Bt
                    mi = 1 if (spn == 0 and grp == 0) else 0
                    pe.wait(tq, tk, spsfree[sb_])
                    pe.e.matmul(sps[sb_][:, 0, :], lhsT=kTt[s][:, kp0:kp0 + (P - 1) * d + 1:d], rhs=qTt[s][:, qsl],
                                start=True, stop=True)
                    ins = pe.e.matmul(sps[sb_][:, 1, :], lhsT=kTt[s][:, kc0:kc0 + (P - 1) * d + 1:d], rhs=qTt[s][:, qsl],
                                      start=True, stop=True)
                    s_tok[i] = pe.mark(ins)
                    act.wait(s_tok[i], pTfree[sb_])
                    t_e = act.mark(act.e.activation(out=pT[sb_][:], in_=sps[sb_][:], func=AF.Exp, scale=scale))
                    spsfree[sb_] = t_e
                    dve.wait(t_e, t_mask)
                    p_tok[i] = dve.mark(dve.e.tensor_tensor(out=pT[sb_][:], in0=pT[sb_][:], in1=mpair[:, mi], op=ALU.mult))

                def emit_PV(i):
                    d, grp, r = blocks[i]
                    Bt = P * d
                    q0 = grp * Bt + r
                    sb_ = (bi + i) % 2
                    pb_ = (bi + i) % NPT
                    vsl, tv = v_tok[i]
                    pe.wait(p_tok[i], tv, olpfree[sb_])
                    pe.e.matmul(olp[sb_][:, 0, :], lhsT=vpc[vsl][:, 0, :], rhs=pT[pb_][:, 0, :], start=True, stop=False)
                    pe.e.matmul(olp[sb_][:, 0, :], lhsT=vpc[vsl][:, 1, :], rhs=pT[pb_][:, 1, :], start=False, stop=True)
                    pe.e.matmul(olp[sb_][:, 1, :], lhsT=ones_b[:], rhs=pT[pb_][:, 0, :], start=True, stop=False)
                    ins = pe.e.matmul(olp[sb_][:, 1, :], lhsT=ones_b[:], rhs=pT[pb_][:, 1, :], start=False, stop=True)
                    t_ol = pe.mark(ins)
                    vfree[vsl] = t_ol
                    pTfree[pb_] = t_ol
                    dst = acc2[:, :, q0:q0 + (P - 1) * d + 1:d]
                    if d == 1:
                        act.wait(t_ol, acc_free)
                        t_acc = act.mark(act.e.copy(out=dst, in_=olp[sb_][:]))
                        copy_toks.append(t_acc)
                    else:
                        dve.wait(t_ol, acc_free, copy_toks, pat_done.get(d))
                        t_acc = dve.mark(dve.e.tensor_tensor(out=dst, in0=olp[sb_][:], in1=dst, op=ALU.add))
                        if d == 4:
                            pat_done[16] = t_acc
                    olpfree[sb_] = t_acc
                    return t_acc

                for i in range(min(NV - 1, nb)):
                    load_v(i)
                emit_S(0)
                emit_S(1)
                t_acc = None
                for i in range(nb):
                    if i + NV - 1 < nb:
                        load_v(i + NV - 1)
                    if i == 20 and u + 1 < len(units):
                        load_qk(u + 1)
                    if i + 2 < nb:
                        emit_S(i + 2)
                    t_acc = emit_PV(i)
                bi += nb
                qkfree[s] = pe.last()
                act.wait(t_acc, rec_free)
                t_r0 = act.mark(act.e.activation(out=rec[:], in_=acc2[:, 1, :], func=AF.Ln))
                act.wait(t_r0)
                t_r = act.mark(act.e.activation(out=rec[:], in_=rec[:], func=AF.Exp, scale=-1.0))
                dve.wait(t_r, t_acc, ostfree[s])
                t_o = dve.mark(dve.e.tensor_tensor(out=ost[s][:], in0=acc2[:, 0, :], in1=rec[:], op=ALU.mult))
                accfree[u % 2] = t_o
                rec_free = t_o
                sp.wait(t_o)
                ostfree[s] = oss[s].dma(sp.e.dma_start(out=yT_s[NH + h, :, spn * 2048:(spn + 1) * 2048], in_=ost[s][:]))
            b.barrier()
        if STOP_AFTER == "B2":
            return b, finish(b, out_d)

        with ExitStack() as p3:
            hn = HeadNorm(p3, "a", gcol)
            oTt = [sb(f"oTt{i}", [P, NH, 512], BF16, p3) for i in range(2)]
            ots = [Slot(b, f"ot_s{i}") for i in range(2)]
            ofree = [None, None]
            tl = {}

            def load_o(st):
                s = st % 2
                sp.wait(ofree[s])
                tl[st] = ots[s].dma(sp.e.dma_start(
                    out=oTt[s][:], in_=yT_s[NH:2 * NH, :, st * 512:(st + 1) * 512].rearrange("h p t -> p h t")))
            load_o(0)
            for st in range(NSTB):
                if st + 1 < NSTB:
                    load_o(st + 1)
                s = st % 2
                ofree[s] = hn.run(st, oTt[s][:], tl[st], NH)
            b.barrier()
    if STOP_AFTER == "B":
        return b, finish(b, out_d)

    with ExitStack() as ph:
        wo = sb("wo", [P, KC, D], BF16, ph)
        gg1 = sb("gg1_bc", [P, D], F32, ph)
        gm2 = sb("gm2_bc", [P, D], F32, ph)
        sh2 = sb("sh2_bc", [P, D], F32, ph)
        yTt = [sb(f"yTt{i}", [P, KC, 256], BF16, ph) for i in range(2)]
        xt = [sb(f"xtC{i}", [P, D], F32, ph) for i in range(2)]
        x1 = [sb(f"x1C{i}", [P, D], F32, ph) for i in range(2)]
        t1 = sb("t1C", [P, D], F32, ph)
        h2 = [sb(f"h2C{i}", [P, D], BF16, ph) for i in range(6)]
        junk = sb("junkC", [P, D], BF16, ph)
        h2T = sb("h2T", [P, KC, P], BF16, ph)
        wrb = sb("wrb", [P, KC, 36], BF16, ph)
        br_bc = sb("br_bc", [P, 36], F32, ph)
        ecap_bc = sb("ecap_bc", [P, NE], F32, ph)
        ustr_f = sb("ustr_f", [P, P], F32, ph)
        ustr_b = sb("ustr_b", [P, P], BF16, ph)
        base = sb("base", [P, NE], F32, ph)
        ssy = sb("ssy", [P, NT, 4], F32, ph)
        st_ = sb("statC", [P, NT, 6], F32, ph)
        rt = sb("rtC", [P, 64], F32, ph)
        L = sb("L", [P, 36], F32, ph)
        le = sb("le", [P, 8], F32, ph)
        le2 = sb("le2", [P, 8], F32, ph)
        oh = sb("oh", [P, 2, 8], F32, ph)
        goh = sb("goh", [P, 4], F32, ph)
        gexp = sb("gexp", [P, 4], F32, ph)
        E = sb("E", [P, 2, NE], F32, ph)
        E12 = sb("E12", [P, NE], BF16, ph)
        posm = sb("posm", [P, NE], F32, ph)
        vm = sb("vm", [P, NE], F32, ph)
        tmp32 = sb("tmp32", [P, NE], F32, ph)
        sidx = [sb(f"sidx{i}", [P, 2], I32, ph) for i in range(2)]
        y2p = [ps(f"y2p{i}", [P, 512], F32, ph) for i in range(4)]
        tpp = [ps(f"tpC{i}", [P, 4, P], BF16, ph) for i in range(2)]
        lgp = ps("lgp", [P, 4, 64], F32, ph)
        prp = ps("prp", [P, 5, NE], F32, ph)
        L4 = sb("L4", [P, 4, 36], F32, ph)
        r4 = sb("r4", [P, 14, 4], F32, ph)
        goh4 = sb("goh4", [P, 4, 4], F32, ph)
        gsh4 = sb("gsh4", [P, 4, 4], F32, ph)
        gex4 = sb("gex4", [P, 4, 4], F32, ph)
        le4 = sb("le4", [P, 4, 8], F32, ph)
        le24 = sb("le24", [P, 4, 8], F32, ph)
        tm8 = sb("tm8", [P, 4, 8], F32, ph)
        oh4 = sb("oh4", [P, 2, 4, 8], F32, ph)
        E4 = sb("E4", [P, 2, 4, NE], F32, ph)
        E124 = sb("E124", [P, 4, NE], BF16, ph)
        posm4 = sb("posm4", [P, 4, NE], F32, ph)
        vm4 = sb("vm4", [P, 4, NE], F32, ph)
        tm32 = sb("tm32", [P, 4, NE], F32, ph)
        w4 = sb("w4", [P, 2, 4], F32, ph)
        dbg4 = sb("dbg4", [P, 4, 4], F32, ph)
        sidx4 = [sb(f"sidx4{i}", [P, 4, 2], I32, ph) for i in range(2)]
        cC = Slot(b, "constC")
        wos = Slot(b, "wo_s")
        yts = [Slot(b, f"yt_s{i}") for i in range(2)]
        xs = [Slot(b, f"xC_s{i}") for i in range(2)]
        x1s = [Slot(b, f"x1C_s{i}") for i in range(2)]
        scs = [Slot(b, f"sc_s{i}") for i in range(2)]

        t_wo = None
        for n in range(4):
            t_wo = wos.dma(pool.e.dma_start(out=wo[:, :, n * 512:(n + 1) * 512],
                                            in_=wout_d[:, n * 512:(n + 1) * 512].rearrange("(k p) f -> p k f", p=P)))
        t_wr = wos.dma(pool.e.dma_start(out=wrb[:], in_=wr_d.rearrange("(k p) f -> p k f", p=P)))
        t_c = [cC.dma(sp.e.dma_start(out=gg1[:], in_=gpost1_d.to_broadcast([P, D]))),
               cC.dma(sp.e.dma_start(out=t1[:], in_=mod_s[0:1, 2 * D:3 * D].to_broadcast([P, D])))]
        dve.wait(*t_c)
        t_gg1 = dve.mark(dve.e.tensor_tensor(out=gg1[:], in0=gg1[:], in1=t1[:], op=ALU.mult))
        sp.wait(t_gg1)
        t_c = [cC.dma(sp.e.dma_start(out=gm2[:], in_=gpre2_d.to_broadcast([P, D]))),
               cC.dma(sp.e.dma_start(out=t1[:], in_=mod_s[0:1, 4 * D:5 * D].to_broadcast([P, D]))),
               cC.dma(sp.e.dma_start(out=sh2[:], in_=mod_s[0:1, 3 * D:4 * D].to_broadcast([P, D]))),
               cC.dma(sp.e.dma_start(out=br_bc[:], in_=br_d.to_broadcast([P, 36]))),
               cC.dma(sp.e.dma_start(out=ecap_bc[:], in_=ecap_d.to_broadcast([P, NE]))),
               cC.dma(sp.e.dma_start(out=ustr_f[:], in_=ustr_d))]
        dve.wait(*t_c)
        dve.mark(dve.e.scalar_tensor_tensor(out=gm2[:], in0=t1[:], scalar=1.0, in1=gm2[:], op0=ALU.add, op1=ALU.mult))
        dve.mark(dve.e.tensor_copy(out=ustr_b[:], in_=ustr_f[:]))
        dve.mark(dve.e.memset(base[:], 0.0))
        dve.mark(dve.e.memset(ssy[:], 0.0))
        t_setup = dve.mark(dve.e.memset(st_[:], 0.0))
        dve.wait(t_setup)

        def dch(ins):
            t = dve.mark(ins)
            dve.wait(t)
            return t

        ytfree = [None, None]
        yt_tok = {}

        def load_yt(st):
            s = st % 2
            sp.wait(ytfree[s])
            yt_tok[st] = yts[s].dma(sp.e.dma_start(
                out=yTt[s][:], in_=yT_s[:, :, st * 256:(st + 1) * 256].rearrange("c p t -> p c t")))

        xfree = [None, None]
        x_tok = {}

        def load_x(t):
            s = t % 2
            sp.wait(xfree[s])
            x_tok[t] = xs[s].dma(sp.e.dma_start(out=xt[s][:], in_=x_d[t * P:(t + 1) * P, :]))

        y2s = [sb(f"y2s{i}", [P, D], F32, ph) for i in range(2)]
        y2free = [None] * 4
        y2sfree = [None, None]
        t1free = t_setup
        x1free = [None, None]
        h2free = [None] * 6
        tpfree = [None, None]
        h2Tfree = None
        lgfree = None
        prfree = None
        sidxfree = [None, None]
        tokA = {}
        tokB = {}

        def st_a(t):
            st, j = t // 2, t % 2
            if j == 0 and st + 1 < NT // 2:
                load_yt(st + 1)
            s = t % 2
            ys = st % 2
            t_y2 = []
            for n in range(4):
                pe.wait(yt_tok[st], t_wo, y2free[n])
                for k in range(KC):
                    ins = pe.e.matmul(y2p[n][:], lhsT=yTt[ys][:, k, j * P:(j + 1) * P], rhs=wo[:, k, n * 512:(n + 1) * 512],
                                      start=(k == 0), stop=(k == KC - 1))
                t_y2.append(pe.mark(ins))
            if j == 1:
                ytfree[ys] = t_y2[-1]
            for n in range(4):
                act.wait(t_y2[n], t_setup, y2sfree[s])
                act.mark(act.e.activation(out=junk[:, n * 512:(n + 1) * 512], in_=y2p[n][:], func=AF.Square,
                                          accum_out=ssy[:, t, n:n + 1]))
                y2free[n] = act.mark(act.e.copy(out=y2s[s][:, n * 512:(n + 1) * 512], in_=y2p[n][:]))
            tokA[t] = act.last()

        tokB1 = {}

        tokB1a = {}
        tokB1b = {}

        def st_b1a(t):
            dve.wait(tokA[t])
            t_s = dch(dve.e.reduce_sum(out=st_[:, t, 0:1], in_=ssy[:, t, :], axis=AX.X))
            tokB1a[t] = rsqrt_small(st_[:, t, 0:1], st_[:, t, 1:2], st_[:, t, 2:3], 1.0 / D, 1, t_s)

        def st_b1b(t):
            s = t % 2
            dve.wait(tokB1a[t], t_gg1, x1free[s])
            t_t1 = dve.mark(dve.e.scalar_tensor_tensor(out=x1[s][:], in0=y2s[s][:], scalar=st_[:, t, 2:3], in1=gg1[:],
                                                       op0=ALU.mult, op1=ALU.mult))
            y2sfree[s] = t_t1
            dve.wait(t_t1, x_tok[t])
            t_x1 = dve.mark(dve.e.tensor_tensor(out=x1[s][:], in0=x1[s][:], in1=xt[s][:], op=ALU.add))
            xfree[s] = t_x1
            sp.wait(t_x1)
            t_x1st = x1s[s].dma(sp.e.dma_start(out=x1_s[t * P:(t + 1) * P, :], in_=x1[s][:]))
            if t + 2 < NT:
                load_x(t + 2)
            act.wait(t_x1)
            t_sq2 = act.mark(act.e.activation(out=junk[:], in_=x1[s][:], func=AF.Square, accum_out=st_[:, t, 3:4]))
            tokB1b[t] = (t_sq2, t_x1st)

        def st_b1c(t):
            t_sq2, t_x1st = tokB1b[t]
            t_rs2 = rsqrt_small(st_[:, t, 3:4], st_[:, t, 4:5], st_[:, t, 5:6], 1.0 / D, 1, t_sq2)
            tokB1[t] = (t_rs2, t_x1st)

        def st_b2(t):
            nonlocal t1free
            s = t % 2
            t_rs2, t_x1st = tokB1[t]
            dve.wait(t_rs2, t1free)
            t_t1b = dve.mark(dve.e.scalar_tensor_tensor(out=t1[:], in0=x1[s][:], scalar=st_[:, t, 5:6], in1=gm2[:],
                                                        op0=ALU.mult, op1=ALU.mult))
            x1free[s] = [t_t1b, t_x1st]
            pool.wait(t_t1b, h2free[t % 6])
            t_h2 = pool.mark(pool.e.tensor_tensor(out=h2[t % 6][:], in0=t1[:], in1=sh2[:], op=ALU.add))
            t1free = t_h2
            tokB[t] = t_h2

        NG = NT // 4
        H2R = 6
        lg_tok = {}
        h2_tok = {}
        tp_tok = {}

        def st_c1(t):
            nonlocal h2Tfree
            s = t % H2R
            j = t % 4
            t_h2 = tokB[t]
            t_evs = []
            for g in range(4):
                pb = g % 2
                pe.wait(t_h2, tpfree[pb])
                for jj in range(4):
                    k = 4 * g + jj
                    ins = pe.e.transpose(out=tpp[pb][:, jj, :], in_=h2[s][:, k * P:(k + 1) * P], identity=ident_b[:])
                t_tp = pe.mark(ins)
                ev = act if g % 2 == 0 else dve
                ev.wait(t_tp, h2Tfree)
                if ev is act:
                    t_ev = act.mark(act.e.copy(out=h2T[:, 4 * g:4 * g + 4, :], in_=tpp[pb][:]))
                else:
                    t_ev = dve.mark(dve.e.tensor_copy(out=h2T[:, 4 * g:4 * g + 4, :], in_=tpp[pb][:]))
                tpfree[pb] = t_ev
                t_evs.append(t_ev)
            pe.wait(t_evs, t_wr)
            if j == 0:
                pe.wait(lgfree)
            for k in range(KC):
                ins = pe.e.matmul(lgp[:, j, 0:36], lhsT=h2T[:, k, :], rhs=wrb[:, k, :], start=(k == 0), stop=(k == KC - 1))
            t_lg = pe.mark(ins)
            h2Tfree = t_lg
            lg_tok[t] = t_lg
            h2_tok[t] = t_h2
            tp_tok[t] = t_tp

        def bc(ap2, n):
            return ap2[:, :, None].to_broadcast([P, 4, n])

        def st_c2(g):
            nonlocal lgfree, prfree
            sg_ = g % 2
            dve.wait(lg_tok[4 * g + 3])
            t_L = dch(dve.e.tensor_tensor(out=L4[:], in0=lgp[:, :, 0:36], in1=br_bc[:, None, :].to_broadcast([P, 4, 36]),
                                          op=ALU.add))
            lgfree = t_L
            dch(dve.e.reduce_max(out=r4[:, 0, :], in_=L4[:, :, 0:4], axis=AX.X))
            dch(dve.e.tensor_tensor(out=goh4[:], in0=L4[:, :, 0:4], in1=bc(r4[:, 0, :], 4), op=ALU.is_equal))
            t_gs = dch(dve.e.tensor_tensor(out=gsh4[:], in0=L4[:, :, 0:4], in1=bc(r4[:, 0, :], 4), op=ALU.subtract))
            act.wait(t_gs)
            t_ge = act.mark(act.e.activation(out=gex4[:], in_=gsh4[:], func=AF.Exp))
            dch(dve.e.tensor_tensor(out=le4[:], in0=L4[:, :, 4:12], in1=bc(goh4[:, :, 0], 8), op=ALU.mult))
            for gg in range(1, 4):
                dch(dve.e.tensor_tensor(out=tm8[:], in0=L4[:, :, 4 + 8 * gg:12 + 8 * gg], in1=bc(goh4[:, :, gg], 8), op=ALU.mult))
                dch(dve.e.tensor_tensor(out=le4[:], in0=le4[:], in1=tm8[:], op=ALU.add))
            dch(dve.e.reduce_max(out=r4[:, 1, :], in_=le4[:], axis=AX.X))
            dch(dve.e.tensor_tensor(out=oh4[:, 0], in0=le4[:], in1=bc(r4[:, 1, :], 8), op=ALU.is_equal))
            dch(dve.e.scalar_tensor_tensor(out=le24[:], in0=oh4[:, 0], scalar=-1e30, in1=le4[:], op0=ALU.mult, op1=ALU.add))
            dch(dve.e.reduce_max(out=r4[:, 2, :], in_=le24[:], axis=AX.X))
            dch(dve.e.tensor_tensor(out=oh4[:, 1], in0=le24[:], in1=bc(r4[:, 2, :], 8), op=ALU.is_equal))
            t_dd = dch(dve.e.tensor_tensor(out=r4[:, 3, :], in0=r4[:, 2, :], in1=r4[:, 1, :], op=ALU.subtract))
            act.wait(t_dd)
            t_ed = act.mark(act.e.activation(out=r4[:, 4, :], in_=r4[:, 3, :], func=AF.Exp))
            for kk in range(2):
                for gg in range(4):
                    ins = dve.e.tensor_tensor(out=E4[:, kk, :, 8 * gg:8 * gg + 8], in0=oh4[:, kk], in1=bc(goh4[:, :, gg], 8),
                                              op=ALU.mult)
            dch(ins)
            dve.wait(prfree)
            t_e12 = dch(dve.e.tensor_tensor(out=E124[:], in0=E4[:, 0], in1=E4[:, 1], op=ALU.add))
            pe.wait(t_e12, t_setup)
            for j in range(4):
                for jp in range(j):
                    pe.e.matmul(prp[:, j, :], lhsT=ones_b[:], rhs=E124[:, jp, :], start=(jp == 0), stop=False)
                pe.e.matmul(prp[:, j, :], lhsT=ustr_b[:], rhs=E124[:, j, :], start=(j == 0), stop=True)
            for j in range(4):
                ins = pe.e.matmul(prp[:, 4, :], lhsT=ones_b[:], rhs=E124[:, j, :], start=(j == 0), stop=(j == 3))
            t_pr = pe.mark(ins)
            dve.wait(t_ge, t_ed)
            dch(dve.e.reduce_sum(out=r4[:, 5, :], in_=gex4[:], axis=AX.X))
            dch(dve.e.reciprocal(out=r4[:, 6, :], in_=r4[:, 5, :]))
            dch(dve.e.tensor_scalar(out=r4[:, 7, :], in0=r4[:, 4, :], scalar1=1.0, scalar2=None, op0=ALU.add))
            dch(dve.e.reciprocal(out=r4[:, 8, :], in_=r4[:, 7, :]))
            dch(dve.e.tensor_tensor(out=w4[:, 0, :], in0=r4[:, 8, :], in1=r4[:, 6, :], op=ALU.mult))
            dch(dve.e.tensor_tensor(out=w4[:, 1, :], in0=r4[:, 4, :], in1=w4[:, 0, :], op=ALU.mult))
            dve.wait(t_pr)
            dch(dve.e.tensor_tensor(out=posm4[:], in0=prp[:, 0:4, :], in1=base[:, None, :].to_broadcast([P, 4, NE]), op=ALU.add))
            t_base = dch(dve.e.tensor_tensor(out=base[:], in0=prp[:, 4, :], in1=base[:], op=ALU.add))
            prfree = t_base
            dch(dve.e.tensor_single_scalar(out=vm4[:], in_=posm4[:], scalar=float(CAP), op=ALU.is_lt))
            dch(dve.e.tensor_tensor(out=posm4[:], in0=posm4[:], in1=ecap_bc[:, None, :].to_broadcast([P, 4, NE]), op=ALU.add))
            dve.wait(sidxfree[sg_])
            for kk in range(2):
                dch(dve.e.tensor_tensor(out=tm32[:], in0=E4[:, kk], in1=posm4[:], op=ALU.mult))
                dch(dve.e.reduce_sum(out=r4[:, 9, :], in_=tm32[:], axis=AX.X))
                dch(dve.e.tensor_tensor(out=tm32[:], in0=E4[:, kk], in1=vm4[:], op=ALU.mult))
                dch(dve.e.reduce_sum(out=r4[:, 10, :], in_=tm32[:], axis=AX.X))
                dch(dve.e.tensor_tensor(out=r4[:, 11, :], in0=r4[:, 9, :], in1=r4[:, 10, :], op=ALU.mult))
                dch(dve.e.tensor_copy(out=ridx[:, 4 * g:4 * g + 4, kk], in_=r4[:, 11, :]))
                dch(dve.e.tensor_tensor(out=rwt[:, 4 * g:4 * g + 4, kk], in0=w4[:, kk, :], in1=r4[:, 10, :], op=ALU.mult))
                dch(dve.e.tensor_scalar(out=r4[:, 12, :], in0=r4[:, 10, :], scalar1=-float(NSLOT), scalar2=float(NSLOT),
                                        op0=ALU.mult, op1=ALU.add))
                dch(dve.e.tensor_tensor(out=r4[:, 13, :], in0=r4[:, 12, :], in1=r4[:, 11, :], op=ALU.add))
                t_si = dch(dve.e.tensor_copy(out=sidx4[sg_][:, :, kk], in_=r4[:, 13, :]))
                if DEBUG:
                    dch(dve.e.tensor_copy(out=dbg4[:, :, 2 + kk], in_=r4[:, 9, :]))
                    dch(dve.e.tensor_copy(out=dbg4[:, :, kk], in_=w4[:, kk, :]))
            for j in range(4):
                t = 4 * g + j
                pool.wait(t_si, h2_tok[t])
                for kk in range(2):
                    t_sc = scs[sg_].dma(pool.e.indirect_dma_start(
                        out=xe_s, out_offset=bass.IndirectOffsetOnAxis(ap=sidx4[sg_][:, j, kk:kk + 1], axis=0),
                        in_=h2[t % H2R][:], in_offset=None))
                h2free[t % H2R] = [t_sc, tp_tok[t]]
            sidxfree[sg_] = t_sc
            if DEBUG:
                sp.wait(dve.last())
                cC.dma(sp.e.dma_start(out=rt_s[:, g * 16:(g + 1) * 16], in_=dbg4[:].rearrange("p j c -> p (j c)")))
                dve.wait(cC.last())

        load_yt(0)
        load_x(0)
        load_x(1)
        for i in range(NT + 3):
            if i < NT:
                st_a(i)
            if 0 <= i - 1 < NT:
                st_b1a(i - 1)
            if 0 <= i - 2 < NT:
                st_b2(i - 2)
            if 0 <= i - 3 < NT:
                st_c1(i - 3)
            if 0 <= i - 1 < NT:
                st_b1b(i - 1)
            if 0 <= i - 3 < NT and (i - 3) % 4 == 3:
                st_c2((i - 3) // 4)
            if 0 <= i - 1 < NT:
                st_b1c(i - 1)
        dve.wait(dve.last())
        dve.mark(dve.e.tensor_copy(out=cnt_i[:], in_=base[:]))
        b.barrier()
    if STOP_AFTER == "D":
        return b, finish(b, out_d)

    with ExitStack() as ph:
        NBE = 5
        wE = [sb(f"wE{i}", [P, 16 * 512], BF16, ph) for i in range(NBE)]
        xe = [sb(f"xe{i}", [P, 4, D], BF16, ph) for i in range(2)]
        xeT = [sb(f"xeT{i}", [P, KC, CAP], BF16, ph) for i in range(2)]
        actT = sb("actT", [P, 8, CAP], BF16, ph)
        sg = [sb(f"sg{i}", [P, CAP], BF16, ph) for i in range(2)]
        yst = [sb(f"yst{i}", [P, 1024], BF16, ph) for i in range(2)]
        tpp = [ps(f"tpE{i}", [P, 4, P], BF16, ph) for i in range(2)]
        gup = [ps(f"gup{i}", [P, 512], F32, ph) for i in range(4)]
        dnp = [ps(f"dnp{i}", [P, 512], F32, ph) for i in range(2)]
        wEs = [Slot(b, f"wE_s{i}") for i in range(NBE)]
        xes = [Slot(b, f"xe_s{i}") for i in range(2)]
        yss = [Slot(b, f"ys_s{i}") for i in range(2)]

        pieces = []
        for e in range(NE):
            for hf in range(2):
                pieces.append((e, "g", hf))
                pieces.append((e, "u", hf))
            for hf in range(2):
                pieces.append((e, "d", hf))
        wfree = [None] * NBE
        w_tok = {}
        wi = 0

        def issue_piece():
            nonlocal wi
            if wi >= len(pieces):
                return
            e, kind, hf = pieces[wi]
            s = wi % NBE
            pool.wait(wfree[s])
            if kind == "g":
                src = wg_d[e, :, hf * 512:(hf + 1) * 512].rearrange("(k p) f -> p k f", p=P)
                dst = wE[s][:].rearrange("p (k f) -> p k f", k=KC)
            elif kind == "u":
                src = wu_d[e, :, hf * 512:(hf + 1) * 512].rearrange("(k p) f -> p k f", p=P)
                dst = wE[s][:].rearrange("p (k f) -> p k f", k=KC)
            else:
                src = wd_d[e, :, hf * 1024:(hf + 1) * 1024].rearrange("(k p) f -> p k f", p=P)
                dst = wE[s][:].rearrange("p (k f) -> p k f", k=8)
            w_tok[(e, kind, hf)] = (s, wEs[s].dma(pool.e.dma_start(out=dst, in_=src)))
            wi += 1

        def issue_piece_st(ST):
            if ST["wi"] >= len(pieces):
                return
            e_, kind, hf = pieces[ST["wi"]]
            s_ = ST["wi"] % NBE
            pool.wait(ST["wfree"][s_])
            if kind == "g":
                src = wg_d[e_, :, hf * 512:(hf + 1) * 512].rearrange("(k p) f -> p k f", p=P)
                dst = wE[s_][:].rearrange("p (k f) -> p k f", k=KC)
            elif kind == "u":
                src = wu_d[e_, :, hf * 512:(hf + 1) * 512].rearrange("(k p) f -> p k f", p=P)
                dst = wE[s_][:].rearrange("p (k f) -> p k f", k=KC)
            else:
                src = wd_d[e_, :, hf * 1024:(hf + 1) * 1024].rearrange("(k p) f -> p k f", p=P)
                dst = wE[s_][:].rearrange("p (k f) -> p k f", k=8)
            ST["w_tok"][(e_, kind, hf)] = (s_, wEs[s_].dma(pool.e.dma_start(out=dst, in_=src)))
            ST["wi"] += 1

        cnt_reg = {x.name: es.enter_context(x.real.register("cnt_" + x.name)) for x in (pe, act, dve)}

        xefree = [None, None]
        xe_tok = {}

        def load_xe(e):
            s = e % 2
            sp.wait(xefree[s])
            xe_tok[e] = xes[s].dma(sp.e.dma_start(
                out=xe[s][:], in_=xe_s[e * CAP:(e + 1) * CAP, :].rearrange("(bb p) f -> p bb f", p=P)))

        for _ in range(NBE):
            issue_piece()
        load_xe(0)
        tpfree = [None, None]
        xeTfree = [None, None]
        gupfree = [None] * 4
        dnpfree = [None, None]
        sgfree = [None, None]
        actfree = None
        ystfree = [None, None]
        gi = 0
        di = 0
        tpi = 0
        for e in range(NE):
            if e + 1 < NE:
                load_xe(e + 1)
            s = e % 2
            t_xT = []
            for bb in range(4):
                for g in range(4):
                    pb = tpi % 2
                    tpi += 1
                    pe.wait(xe_tok[e], tpfree[pb])
                    for jj in range(4):
                        k = 4 * g + jj
                        ins = pe.e.transpose(out=tpp[pb][:, jj, :], in_=xe[s][:, bb, k * P:(k + 1) * P], identity=ident_b[:])
                    t_tp = pe.mark(ins)
                    ev = act if (tpi % 2 == 0) else dve
                    ev.wait(t_tp, xeTfree[s])
                    if ev is act:
                        t_ev = act.mark(act.e.copy(out=xeT[s][:, 4 * g:4 * g + 4, bb * P:(bb + 1) * P], in_=tpp[pb][:]))
                    else:
                        t_ev = dve.mark(dve.e.tensor_copy(out=xeT[s][:, 4 * g:4 * g + 4, bb * P:(bb + 1) * P], in_=tpp[pb][:]))
                    tpfree[pb] = t_ev
                    t_xT.append(t_ev)
            xefree[s] = t_tp
            ST = dict(gi=gi, gupfree=list(gupfree), sgfree=list(sgfree), wfree=list(wfree), wi=wi,
                      w_tok=dict(w_tok), t_act_all=[])

            def gate_up(nblk, ST):
                N = nblk * P
                for hf in range(2):
                    sgw, t_gw = ST["w_tok"][(e, "g", hf)]
                    suw, t_uw = ST["w_tok"][(e, "u", hf)]
                    gv_ = wE[sgw][:].rearrange("p (k f) -> p k f", k=KC)
                    uv_ = wE[suw][:].rearrange("p (k f) -> p k f", k=KC)
                    for m in range(4):
                        j = hf * 4 + m
                        pg = ST["gi"] % 4
                        pu = (ST["gi"] + 1) % 4
                        ST["gi"] += 2
                        pe.wait(t_gw, t_uw, t_xT, ST["gupfree"][pg], ST["gupfree"][pu])
                        for k in range(KC):
                            ins = pe.e.matmul(gup[pg][:, 0:N], lhsT=gv_[:, k, m * P:(m + 1) * P], rhs=xeT[s][:, k, 0:N],
                                              start=(k == 0), stop=(k == KC - 1))
                        t_g = pe.mark(ins)
                        for k in range(KC):
                            ins = pe.e.matmul(gup[pu][:, 0:N], lhsT=uv_[:, k, m * P:(m + 1) * P], rhs=xeT[s][:, k, 0:N],
                                              start=(k == 0), stop=(k == KC - 1))
                        t_u = pe.mark(ins)
                        ss_ = j % 2
                        act.wait(t_g, ST["sgfree"][ss_])
                        t_sg = act.mark(act.e.activation(out=sg[ss_][:, 0:N], in_=gup[pg][:, 0:N], func=AF.Silu))
                        ST["gupfree"][pg] = t_sg
                        dve.wait(t_sg, t_u)
                        if j == 0:
                            dve.wait(actfree)
                        t_a = dve.mark(dve.e.tensor_tensor(out=actT[:, j, 0:N], in0=gup[pu][:, 0:N], in1=sg[ss_][:, 0:N],
                                                           op=ALU.mult))
                        ST["gupfree"][pu] = t_a
                        ST["sgfree"][ss_] = t_a
                        ST["t_act_all"].append(t_a)
                    ST["wfree"][sgw] = pe.last()
                    ST["wfree"][suw] = pe.last()
                    for _ in range(2):
                        issue_piece_st(ST)

            def snap_all():
                return ([(x.n, dict(x.seen)) for x in b.engs], [sl.n for sl in b.slots])

            def restore_all(sn):
                for x, (n_, seen_) in zip(b.engs, sn[0]):
                    x.n = n_
                    x.seen = dict(seen_)
                for sl, n_ in zip(b.slots, sn[1]):
                    sl.n = n_

            def copy_ST(S0):
                return dict(gi=S0["gi"], gupfree=list(S0["gupfree"]), sgfree=list(S0["sgfree"]), wfree=list(S0["wfree"]),
                            wi=S0["wi"], w_tok=dict(S0["w_tok"]), t_act_all=[])

            sn0 = snap_all()
            ST_end = None
            for y_ in b.engs:
                y_.muted = True
            pool.muted = False
            restore_all(sn0)
            ST_end = copy_ST(ST)
            gate_up(4, ST_end)
            for X in (pe, act, dve):
                for y_ in b.engs:
                    y_.muted = True
                X.muted = False
                X.real.reg_load(cnt_reg[X.name], cnt_i[0:1, e:e + 1])
                with X.real.If_lt(cnt_reg[X.name], 2 * P + 1):
                    restore_all(sn0)
                    ST_end = copy_ST(ST)
                    gate_up(2, ST_end)
                with X.real.Else():
                    with X.real.If_lt(cnt_reg[X.name], 3 * P + 1):
                        restore_all(sn0)
                        ST_end = copy_ST(ST)
                        gate_up(3, ST_end)
                    with X.real.Else():
                        restore_all(sn0)
                        ST_end = copy_ST(ST)
                        gate_up(4, ST_end)
            for y_ in b.engs:
                y_.muted = False
            gi = ST_end["gi"]
            gupfree = ST_end["gupfree"]
            sgfree = ST_end["sgfree"]
            wfree = ST_end["wfree"]
            wi = ST_end["wi"]
            w_tok = ST_end["w_tok"]
            t_act_all = ST_end["t_act_all"]
            xeTfree[s] = pe.last()
            for hf in range(2):
                sdw, t_dw = w_tok[(e, "d", hf)]
                dv_ = wE[sdw][:].rearrange("p (k f) -> p k f", k=8)
                for bb in range(4):
                    ys_ = di % 2
                    di += 1
                    t_ev = None
                    for n2 in range(2):
                        pd = (2 * di + n2) % 2
                        pe.wait(t_dw, t_act_all, dnpfree[pd])
                        for jk in range(8):
                            ins = pe.e.matmul(dnp[pd][:], lhsT=actT[:, jk, bb * P:(bb + 1) * P],
                                              rhs=dv_[:, jk, n2 * 512:(n2 + 1) * 512], start=(jk == 0), stop=(jk == 7))
                        t_d = pe.mark(ins)
                        ev = act if n2 == 0 else dve
                        ev.wait(t_d, ystfree[ys_])
                        if ev is act:
                            t_e = act.mark(act.e.copy(out=yst[ys_][:, n2 * 512:(n2 + 1) * 512], in_=dnp[pd][:]))
                        else:
                            t_e = dve.mark(dve.e.tensor_copy(out=yst[ys_][:, n2 * 512:(n2 + 1) * 512], in_=dnp[pd][:]))
                        dnpfree[pd] = t_e
                        sp.wait(t_e)
                    r0 = e * CAP + bb * P
                    ystfree[ys_] = yss[ys_].dma(sp.e.dma_start(out=ye_s[r0:r0 + P, hf * 1024:(hf + 1) * 1024], in_=yst[ys_][:]))
                wfree[sdw] = pe.last()
                issue_piece()
            actfree = pe.last()
        b.barrier()
    if STOP_AFTER == "E":
        return b, finish(b, out_d)

    with ExitStack() as ph:
        gg2 = sb("gg2_bc", [P, D], F32, ph)
        tF = sb("tF", [P, D], F32, ph)
        Y = [[sb(f"Y{i}{k}", [P, D], BF16, ph) for k in range(2)] for i in range(2)]
        x1t = [sb(f"x1F{i}", [P, D], F32, ph) for i in range(2)]
        ym = [sb(f"ymF{i}", [P, D], F32, ph) for i in range(2)]
        ot = [sb(f"otF{i}", [P, D], F32, ph) for i in range(2)]
        junk = sb("junkF", [P, D], BF16, ph)
        stF = sb("stF", [P, NT, 3], F32, ph)
        cF = Slot(b, "constF")
        gs = [[Slot(b, f"gF_s{i}{k}") for k in range(2)] for i in range(2)]
        x1s = [Slot(b, f"x1F_s{i}") for i in range(2)]
        os_ = [Slot(b, f"oF_s{i}") for i in range(2)]
        t_c = [cF.dma(sp.e.dma_start(out=gg2[:], in_=gpost2_d.to_broadcast([P, D]))),
               cF.dma(sp.e.dma_start(out=tF[:], in_=mod_s[0:1, 5 * D:6 * D].to_broadcast([P, D])))]
        dve.wait(*t_c)
        t_gg2 = dve.mark(dve.e.tensor_tensor(out=gg2[:], in0=gg2[:], in1=tF[:], op=ALU.mult))
        t_z = dve.mark(dve.e.memset(stF[:], 0.0))
        Yfree = [None, None]
        x1free = [None, None]
        ymfree = [None, None]
        otfree = [None, None]
        tFfree = t_gg2
        ld = {}

        def loadY(t):
            s = t % 2
            pool.wait(Yfree[s])
            ld[t] = [gs[s][k].dma(pool.e.indirect_dma_start(
                out=Y[s][k][:], out_offset=None, in_=ye_s,
                in_offset=bass.IndirectOffsetOnAxis(ap=ridx[:, t, k:k + 1], axis=0))) for k in range(2)]

        ldx = {}

        def loadX(t):
            s = t % 2
            sp.wait(x1free[s])
            ldx[t] = x1s[s].dma(sp.e.dma_start(out=x1t[s][:], in_=x1_s[t * P:(t + 1) * P, :]))

        tokF = {}

        def stF1(t):
            s = t % 2
            tg = ld[t]
            act.wait(tg[0], ymfree[s])
            t_a = act.mark(act.e.activation(out=ym[s][:], in_=Y[s][0][:], func=AF.Identity, scale=rwt[:, t, 0:1]))
            dve.wait(t_a, tg)
            t_b = dve.mark(dve.e.scalar_tensor_tensor(out=ym[s][:], in0=Y[s][1][:], scalar=rwt[:, t, 1:2], in1=ym[s][:],
                                                      op0=ALU.mult, op1=ALU.add))
            Yfree[s] = t_b
            if t + 2 < NT:
                loadY(t + 2)
            act.wait(t_b, t_z)
            t_sq = act.mark(act.e.activation(out=junk[:], in_=ym[s][:], func=AF.Square, accum_out=stF[:, t, 0:1]))
            tokF[t] = rsqrt_small(stF[:, t, 0:1], stF[:, t, 1:2], stF[:, t, 2:3], 1.0 / D, 1, t_sq)

        def stF2(t):
            nonlocal tFfree
            s = t % 2
            tx = ldx[t]
            dve.wait(tokF[t], tFfree)
            t_t = dve.mark(dve.e.scalar_tensor_tensor(out=tF[:], in0=ym[s][:], scalar=stF[:, t, 2:3], in1=gg2[:],
                                                      op0=ALU.mult, op1=ALU.mult))
            ymfree[s] = t_t
            HF = D // 2
            pool.wait(t_t, tx, otfree[s])
            t_o1 = pool.mark(pool.e.tensor_tensor(out=ot[s][:, 0:HF], in0=tF[:, 0:HF], in1=x1t[s][:, 0:HF], op=ALU.add))
            dve.wait(t_t, tx, otfree[s])
            t_o2 = dve.mark(dve.e.tensor_tensor(out=ot[s][:, HF:D], in0=tF[:, HF:D], in1=x1t[s][:, HF:D], op=ALU.add))
            t_o = [t_o1, t_o2]
            tFfree = t_o
            x1free[s] = t_o
            sp.wait(t_o)
            otfree[s] = os_[s].dma(sp.e.dma_start(out=out_d[t * P:(t + 1) * P, :], in_=ot[s][:]))
            if t + 2 < NT:
                loadX(t + 2)

        loadY(0)
        loadY(1)
        loadX(0)
        loadX(1)
        for i in range(NT + 1):
            if i < NT:
                stF1(i)
            if 0 <= i - 1 < NT:
                stF2(i - 1)
    finish(b, out_d)
    return b, None


def finish(b, out_d):
    b.barrier()
    return None


_CACHE = {}


def _consts(half):
    ident = np.eye(P, dtype=np.float32)
    tril = np.tril(np.ones((P, P), np.float32))
    ustrict = np.triu(np.ones((P, P), np.float32), 1)
    j = np.arange(P)[:, None]
    i = np.arange(P)[None, :]
    cur = np.where(j <= i, 0.0, NEG).astype(np.float32)
    prev = np.where(j >= i, 0.0, NEG).astype(np.float32)
    prev_halo = prev if half == 1 else np.full((P, P), NEG, np.float32)
    maskb = np.stack([cur, prev, prev_halo], axis=1)
    ecap = (np.arange(NE, dtype=np.float32) * CAP)[None, :]
    return dict(ident=ident, tril=tril, ustrict=ustrict, maskb=np.ascontiguousarray(maskb), ecap=ecap)


def make_in_maps(x, c, w_mod, b_mod, g_pre_mix, g_post_mix, w_in, g_gmlp_v, w_spatial,
                 b_spatial, g_out_gmlp, g_out_attn, w_out, g_pre_ffn, g_post_ffn,
                 w_router_group, b_router_group, w_router_expert, b_router_expert,
                 w_gate, w_up, w_down):
    f = lambda a: np.ascontiguousarray(np.asarray(a, dtype=np.float32))
    shared = dict(
        w_mod=f(w_mod[0]), b_mod=f(b_mod[0][None, :]),
        g_pre_mix=f(g_pre_mix[0][None, :]), g_post_mix=f(g_post_mix[0][None, :]),
        g_pre_ffn=f(g_pre_ffn[0][None, :]), g_post_ffn=f(g_post_ffn[0][None, :]),
        g_gmlp_v=f(g_gmlp_v[0][None, :]),
        g_out=f(np.concatenate([g_out_gmlp[0], g_out_attn[0]]).reshape(KC, P).T),
        w_in=f(w_in[0]), w_out=f(w_out[0]),
        w_sp=f(w_spatial[0]), b_sp=f(b_spatial[0].reshape(1, NH * P)),
        w_r=f(np.concatenate([w_router_group[0], np.transpose(w_router_expert[0], (1, 0, 2)).reshape(D, 32)], axis=1)),
        b_r=f(np.concatenate([b_router_group[0], b_router_expert[0].reshape(32)])[None, :]),
        w_gate=f(w_gate[0]), w_up=f(w_up[0]), w_down=f(w_down[0]),
    )
    maps = []
    x = np.asarray(x, dtype=np.float32)
    for core in range(8):
        bi, half = core // 2, core % 2
        m = dict(shared)
        m["x"] = f(x[bi, half * TOWN:(half + 1) * TOWN])
        m["xh"] = f(x[bi, 2048:4096]) if half == 1 else f(x[bi, 0:2048])
        m["c"] = f(np.asarray(c[bi], dtype=np.float32).reshape(KC, P).T)
        m.update(_consts(half))
        maps.append(m)
    return maps


def kernel(**inputs):
    if "nc" not in _CACHE:
        b, _ = build()
        _CACHE["nc"] = b.nc
    nc = _CACHE["nc"]
    maps = make_in_maps(**inputs)
    res = run_bass_kernel_spmd(nc, maps, core_ids=list(range(8)))
    out = np.empty((4, 8192, D), np.float32)
    for core in range(8):
        bi, half = core // 2, core % 2
        out[bi, half * TOWN:(half + 1) * TOWN] = res.results[core]["out"]
    if DEBUG:
        _CACHE["res"] = res
    return out
```

```python
import os
from contextlib import ExitStack

import numpy as np
import concourse.bass as bass
import concourse.mybir as mybir
from concourse.bass_utils import run_bass_kernel_spmd

F32, BF16, I32 = mybir.dt.float32, mybir.dt.bfloat16, mybir.dt.int32
AF = mybir.ActivationFunctionType
ALU = mybir.AluOpType
AX = mybir.AxisListType

P = 128
D = 2048
KC = 16
TOWN = 4096
THALO = 2048
TEXT = TOWN + THALO
DG = 1024
NH = 8
DIN = 5120
NE = 32
DE = 1024
CAP = 512
NSLOT = NE * CAP
EPS = 1e-6
NEG = -30000.0
NT = TOWN // P

STOP_AFTER = os.environ.get("MK_STOP_AFTER", "")
DEBUG = bool(STOP_AFTER)


class _DummyIns:
    def then_inc(self, *a, **k):
        return self


class _DummyEngine:
    def __getattr__(self, name):
        def f(*a, **k):
            return _DummyIns()
        return f


_DUMMY = _DummyEngine()


class Eng:
    def __init__(self, b, e, name):
        self.b, self.real, self.name = b, e, name
        self.sem = b.newsem(name + "_prog")
        self.n = 0
        self.seen = {}
        self.muted = False

    @property
    def e(self):
        return _DUMMY if self.muted else self.real

    def wait(self, *toks):
        for t in toks:
            if t is None:
                continue
            if isinstance(t, list):
                self.wait(*t)
                continue
            sem, val = t
            if self.seen.get(sem.num, 0) < val:
                if not self.muted:
                    self.real.wait_ge(sem, val)
                self.seen[sem.num] = val

    def mark(self, ins):
        self.n += 1
        ins.then_inc(self.sem, 1)
        return (self.sem, self.n)

    def last(self):
        return (self.sem, self.n) if self.n else None


class Slot:
    def __init__(self, b, name):
        self.sem = b.newsem(name)
        self.n = 0
        b.slots.append(self)

    def dma(self, ins):
        self.n += 16
        ins.then_inc(self.sem, 16)
        return (self.sem, self.n)

    def last(self):
        return (self.sem, self.n) if self.n else None


class Builder:
    def __init__(self):
        self.nc = bass.Bass("TRN2", target_bir_lowering=False)
        self.es = ExitStack()
        self.slots = []
        self._semn = 0
        nc = self.nc
        self.pe = Eng(self, nc.tensor, "pe")
        self.act = Eng(self, nc.scalar, "act")
        self.dve = Eng(self, nc.vector, "dve")
        self.pool = Eng(self, nc.gpsimd, "pool")
        self.sp = Eng(self, nc.sync, "sp")
        self.engs = [self.pe, self.act, self.dve, self.pool, self.sp]

    def newsem(self, name):
        self._semn += 1
        return self.es.enter_context(self.nc.semaphore(f"{name}_{self._semn}"))

    def barrier(self):
        toks = [e.last() for e in self.engs] + [s.last() for s in self.slots]
        for e in self.engs:
            e.wait(*toks)


def build():
    b = Builder()
    nc = b.nc
    pe, act, dve, pool, sp = b.pe, b.act, b.dve, b.pool, b.sp

    def din(name, shape, dt=F32):
        return nc.dram_tensor(name, list(shape), dt, kind="ExternalInput").ap()

    def dscr(name, shape, dt, dbg=False):
        kind = "ExternalOutput" if (DEBUG and dbg) else "Internal"
        return nc.dram_tensor(name, list(shape), dt, kind=kind).ap()

    x_d = din("x", [TOWN, D])
    xh_d = din("xh", [THALO, D])
    c_d = din("c", [P, KC])
    wmod_d = din("w_mod", [D, 6 * D])
    bmod_d = din("b_mod", [1, 6 * D])
    gpre1_d = din("g_pre_mix", [1, D])
    gpost1_d = din("g_post_mix", [1, D])
    gpre2_d = din("g_pre_ffn", [1, D])
    gpost2_d = din("g_post_ffn", [1, D])
    ggv_d = din("g_gmlp_v", [1, DG])
    gout_d = din("g_out", [P, KC])
    win_d = din("w_in", [D, DIN])
    wout_d = din("w_out", [D, D])
    wsp_d = din("w_sp", [NH, P, P])
    bsp_d = din("b_sp", [1, NH * P])
    wr_d = din("w_r", [D, 36])
    br_d = din("b_r", [1, 36])
    wg_d = din("w_gate", [NE, D, DE])
    wu_d = din("w_up", [NE, D, DE])
    wd_d = din("w_down", [NE, DE, D])
    ident_d = din("ident", [P, P])
    tril_d = din("tril", [P, P])
    ustr_d = din("ustrict", [P, P])
    maskb_d = din("maskb", [P, 3, P])
    ecap_d = din("ecap", [1, NE])
    out_d = nc.dram_tensor("out", [TOWN, D], F32, kind="ExternalOutput").ap()

    mod_s = dscr("mod_s", [1, 6 * D], F32, True)
    uT_s = dscr("uT_s", [NH, P, TOWN], BF16, True)
    gv_s = dscr("gv_s", [TOWN, DG], BF16, True)
    qT_s = dscr("qT_s", [NH, P, TOWN], BF16, True)
    kT_s = dscr("kT_s", [NH, P, TEXT], BF16, True)
    v_s = dscr("v_s", [TEXT, DG], BF16, True)
    yT_s = dscr("yT_s", [KC, P, TOWN], BF16, True)
    x1_s = dscr("x1_s", [TOWN, D], F32, True)
    xe_s = dscr("xe_s", [NSLOT + P, D], BF16, False)
    ye_s = dscr("ye_s", [NSLOT, D], BF16, False)
    rt_s = dscr("rt_s", [P, NT * 4], F32, True)

    es = b.es

    def sb(name, shape, dt, stack=None):
        return (stack or es).enter_context(nc.sbuf_tensor("sb_" + name, list(shape), dt))

    def ps(name, shape, dt, stack=None):
        return (stack or es).enter_context(nc.psum_tensor("ps_" + name, list(shape), dt))

    ident_f = sb("ident_f", [P, P], F32)
    ident_b = sb("ident_b", [P, P], BF16)
    ones_b = sb("ones_b", [P, P], BF16)
    neghalf = sb("neghalf", [P, 512], F32)
    eps_t = sb("eps_t", [P, 1], F32)
    lnst = sb("lnst", [P, NT, 4], F32)
    ridx = sb("ridx", [P, NT, 2], I32)
    rwt = sb("rwt", [P, NT, 2], F32)
    cnt_i = sb("cnt_i", [P, NE], I32)

    ld0 = Slot(b, "ld0")
    t_id = ld0.dma(sp.e.dma_start(out=ident_f[:], in_=ident_d))
    dve.wait(t_id)
    dve.mark(dve.e.tensor_copy(out=ident_b[:], in_=ident_f[:]))
    dve.mark(dve.e.memset(ones_b[:], 1.0))
    dve.mark(dve.e.memset(neghalf[:], -0.5))
    dve.mark(dve.e.memset(lnst[:], 0.0))
    dve.mark(dve.e.memset(eps_t[:], EPS))
    t_const = dve.last()

    def rsqrt_small(ss_ap, ms_ap, out_ap, scale, n, after):
        dve.wait(after)
        t = dve.mark(dve.e.tensor_scalar(out=ms_ap, in0=ss_ap, scalar1=scale, scalar2=EPS,
                                         op0=ALU.mult, op1=ALU.add))
        pool.wait(t, t_const)
        return pool.mark(pool.e.tensor_tensor(out=out_ap, in0=ms_ap, in1=neghalf[:, 0:n], op=ALU.pow))

    sc = sb("sc", [P, KC], BF16)
    NPC = 6 * D // 512
    NPC0 = 8

    class ModCalc:
        def __init__(self, ph, tag):
            self.NB = 3
            self.wm = [sb(f"wm{tag}{i}", [P, KC, 512], BF16, ph) for i in range(self.NB)]
            self.bp = [sb(f"bp{tag}{i}", [1, 512], F32, ph) for i in range(self.NB)]
            self.mp = [sb(f"mp{tag}{i}", [1, 512], F32, ph) for i in range(2)]
            self.mps = [ps(f"mps{tag}{i}", [1, 512], F32, ph) for i in range(2)]
            self.ws = [Slot(b, f"wm{tag}_s{i}") for i in range(self.NB)]
            self.bs = [Slot(b, f"bp{tag}_s{i}") for i in range(self.NB)]
            self.ms = [Slot(b, f"mp{tag}_s{i}") for i in range(2)]
            self.wfree = [None] * self.NB
            self.bfree = [None] * self.NB
            self.mfree = [None, None]
            self.psfree = [None, None]
            self.tok = {}
            self.ni = 0
            self.nc_ = 0

        def issue(self, j):
            s = self.ni % self.NB
            s2 = self.ni % 2
            self.ni += 1
            pool.wait(self.wfree[s])
            tw = self.ws[s].dma(pool.e.dma_start(
                out=self.wm[s][:], in_=wmod_d[:, j * 512:(j + 1) * 512].rearrange("(k p) f -> p k f", p=P)))
            sp.wait(self.bfree[s])
            tb = self.bs[s].dma(sp.e.dma_start(out=self.bp[s][:], in_=bmod_d[0:1, j * 512:(j + 1) * 512]))
            self.tok[j] = (s, s2, tw, tb)

        def compute(self, j):
            s, s2, tw, tb = self.tok[j]
            pe.wait(tw, t_sc, self.psfree[s2])
            for k in range(KC):
                ins = pe.e.matmul(self.mps[s2][:], lhsT=sc[:, k:k + 1], rhs=self.wm[s][:, k, :],
                                  start=(k == 0), stop=(k == KC - 1))
            t_pe = pe.mark(ins)
            self.wfree[s] = t_pe
            dve.wait(t_pe, tb, self.mfree[s2])
            t_ev = dve.mark(dve.e.tensor_tensor(out=self.mp[s2][:], in0=self.mps[s2][:], in1=self.bp[s][:], op=ALU.add))
            self.psfree[s2] = t_ev
            self.bfree[s] = t_ev
            sp.wait(t_ev)
            self.mfree[s2] = self.ms[s2].dma(sp.e.dma_start(out=mod_s[0:1, j * 512:(j + 1) * 512], in_=self.mp[s2][:]))

    with ExitStack() as ph:
        c_t = sb("c_t", [P, KC], F32, ph)
        t_c = ld0.dma(sp.e.dma_start(out=c_t[:], in_=c_d))
        act.wait(t_c)
        t_sc = act.mark(act.e.activation(out=sc[:], in_=c_t[:], func=AF.Silu))
        mc = ModCalc(ph, "0")
        for j in range(3):
            mc.issue(j)
        for j in range(NPC0):
            mc.compute(j)
            if j + 3 < NPC0:
                mc.issue(j + 3)
        b.barrier()
    if STOP_AFTER == "0":
        return b, finish(b, out_d)

    with ExitStack() as ph:
        gm_bc = sb("gm1_bc", [P, D], F32, ph)
        sh_bc = sb("sh1_bc", [P, D], F32, ph)
        hT = sb("hT", [P, KC, 2048], BF16, ph)
        xt = [sb(f"xtA{i}", [P, D], F32, ph) for i in range(2)]
        t1 = sb("t1A", [P, D], F32, ph)
        xn = [sb(f"xnA{i}", [P, D], BF16, ph) for i in range(2)]
        junk = sb("junkA", [P, D], BF16, ph)
        ssA = sb("ssA", [P, 48], F32, ph)
        msA = sb("msA", [P, 48], F32, ph)
        rsA = sb("rsA", [P, 48], F32, ph)
        NBW = 3
        wr_ = [sb(f"wA{i}", [P, KC, 512], BF16, ph) for i in range(NBW)]
        NST = 4
        stg = [sb(f"stgA{i}", [P, 512], BF16, ph) for i in range(NST)]
        sqj = sb("sqjA", [P, 512], BF16, ph)
        tpp = [ps(f"tpA{i}", [P, 4, P], BF16, ph) for i in range(2)]
        mmp = [ps(f"mmA{i}", [P, 512], F32, ph) for i in range(4)]

        xs = [Slot(b, f"xA_s{i}") for i in range(2)]
        ws = [Slot(b, f"wA_s{i}") for i in range(NBW)]
        sts = [Slot(b, f"stA_s{i}") for i in range(NST)]
        cs = Slot(b, "constA")

        t_a = cs.dma(sp.e.dma_start(out=gm_bc[:], in_=gpre1_d.to_broadcast([P, D])))
        t_b2 = cs.dma(sp.e.dma_start(out=t1[:], in_=mod_s[0:1, D:2 * D].to_broadcast([P, D])))
        t_c2 = cs.dma(sp.e.dma_start(out=sh_bc[:], in_=mod_s[0:1, 0:D].to_broadcast([P, D])))
        dve.wait(t_a, t_b2, t_c2)
        t_gm = dve.mark(dve.e.scalar_tensor_tensor(out=gm_bc[:], in0=t1[:], scalar=1.0, in1=gm_bc[:],
                                                   op0=ALU.add, op1=ALU.mult))
        t_ssz = dve.mark(dve.e.memset(ssA[:], 0.0))

        spans = [("halo", xh_d, 0, [6, 7, 8, 9]), ("own0", x_d, 0, list(range(10))),
                 ("own1", x_d, 2048, list(range(10)))]
        gtile = 0
        xfree = [None, None]
        t1free = t_gm
        xnfree = [None, None]
        tpfree = [None, None]
        mmfree = [None] * 4
        stfree = [None] * NST
        wfree = [None] * NBW
        mmi = 0
        sti = 0
        wi = 0
        hT_readers = None

        for (sname, xsrc, xoff, blocks) in spans:
            is_halo = sname == "halo"
            ext_off = 0 if is_halo else (2048 + xoff)
            w_tok = {}
            pend = list(blocks)

            def issue_wA(blk):
                nonlocal wi
                s = wi % NBW
                wi += 1
                pool.wait(wfree[s])
                w_tok[blk] = (s, ws[s].dma(pool.e.dma_start(
                    out=wr_[s][:], in_=win_d[:, blk * 512:(blk + 1) * 512].rearrange("(k p) f -> p k f", p=P))))

            for _ in range(min(NBW, len(pend))):
                issue_wA(pend.pop(0))

            hT_ready = []
            xn_tok = {}

            def stage_X(i):
                nonlocal gtile, t1free
                s2 = gtile % 2
                row0 = xoff + i * P
                sp.wait(xfree[s2])
                t_x = xs[s2].dma(sp.e.dma_start(out=xt[s2][:], in_=xsrc[row0:row0 + P, :]))
                act.wait(t_x, t_ssz)
                t_ss = act.mark(act.e.activation(out=junk[:], in_=xt[s2][:], func=AF.Square,
                                                 accum_out=ssA[:, gtile:gtile + 1]))
                t_rs = rsqrt_small(ssA[:, gtile:gtile + 1], msA[:, gtile:gtile + 1], rsA[:, gtile:gtile + 1],
                                   1.0 / D, 1, t_ss)
                dve.wait(t_rs, t_x, t1free, t_gm)
                t_t1 = dve.mark(dve.e.scalar_tensor_tensor(out=t1[:], in0=xt[s2][:], scalar=rsA[:, gtile:gtile + 1],
                                                           in1=gm_bc[:], op0=ALU.mult, op1=ALU.mult))
                xfree[s2] = t_t1
                pool.wait(t_t1, xnfree[s2], t_c2)
                t_xn = pool.mark(pool.e.tensor_tensor(out=xn[s2][:], in0=t1[:], in1=sh_bc[:], op=ALU.add))
                t1free = t_xn
                xn_tok[i] = (s2, t_xn)
                gtile += 1

            def stage_Y(i):
                s2, t_xn = xn_tok[i]
                for g in range(4):
                    pb = g % 2
                    pe.wait(t_xn, tpfree[pb])
                    if i == 0:
                        pe.wait(hT_readers)
                    for j in range(4):
                        k = 4 * g + j
                        ins = pe.e.transpose(out=tpp[pb][:, j, :], in_=xn[s2][:, k * P:(k + 1) * P],
                                             identity=ident_b[:])
                    t_tp = pe.mark(ins)
                    ev = act if g % 2 == 0 else dve
                    ev.wait(t_tp)
                    if i == 0:
                        ev.wait(hT_readers)
                    if ev is act:
                        t_ev = act.mark(act.e.copy(out=hT[:, 4 * g:4 * g + 4, i * P:(i + 1) * P], in_=tpp[pb][:]))
                    else:
                        t_ev = dve.mark(dve.e.tensor_copy(out=hT[:, 4 * g:4 * g + 4, i * P:(i + 1) * P],
                                                          in_=tpp[pb][:]))
                    tpfree[pb] = t_ev
                    hT_ready.append(t_ev)
                xnfree[s2] = t_tp

            stage_X(0)
            for i in range(16):
                if i + 1 < 16:
                    stage_X(i + 1)
                stage_Y(i)

            def stage_out(src_ps, kind, dst_ap, pe_tok, extra=None):
                nonlocal sti
                s = sti % NST
                sti += 1
                if kind == "gelu":
                    act.wait(pe_tok, stfree[s])
                    if extra is not None:
                        t_e = act.mark(act.e.activation(out=stg[s][:], in_=src_ps[:], func=AF.Gelu,
                                                        accum_out=extra[0]))
                        dve.wait(t_e)
                        t_q = dve.mark(dve.e.tensor_tensor(out=sqj[:], in0=stg[s][:], in1=stg[s][:], op=ALU.mult))
                        dve.wait(t_q)
                        t_q2 = dve.mark(dve.e.reduce_sum(out=extra[1], in_=sqj[:], axis=AX.X))
                    else:
                        t_e = act.mark(act.e.activation(out=stg[s][:], in_=src_ps[:], func=AF.Gelu))
                else:
                    dve.wait(pe_tok, stfree[s])
                    t_e = dve.mark(dve.e.tensor_copy(out=stg[s][:], in_=src_ps[:]))
                sp.wait(t_e)
                stfree[s] = [sts[s].dma(sp.e.dma_start(out=dst_ap, in_=stg[s][:]))]
                if kind == "gelu" and extra is not None:
                    stfree[s].append(t_q)
                return t_e

            for blk in blocks:
                (s, t_w) = w_tok[blk]
                fm = blk in (0, 1, 4, 5, 6, 7)
                if fm:
                    for m in range(4):
                        head = (blk % 2) * 4 + m
                        for st in range(4):
                            pb = mmi % 4
                            mmi += 1
                            pe.wait(t_w, hT_ready, mmfree[pb])
                            for k in range(KC):
                                ins = pe.e.matmul(mmp[pb][:], lhsT=wr_[s][:, k, m * P:(m + 1) * P],
                                                  rhs=hT[:, k, st * 512:(st + 1) * 512],
                                                  start=(k == 0), stop=(k == KC - 1))
                            t_mm = pe.mark(ins)
                            if blk in (0, 1):
                                dst = uT_s[head, :, xoff + st * 512: xoff + (st + 1) * 512]
                                mmfree[pb] = stage_out(mmp[pb], "gelu", dst, t_mm)
                            elif blk in (4, 5):
                                dst = qT_s[head, :, xoff + st * 512: xoff + (st + 1) * 512]
                                mmfree[pb] = stage_out(mmp[pb], "copy", dst, t_mm)
                            else:
                                dst = kT_s[head, :, ext_off + st * 512: ext_off + (st + 1) * 512]
                                mmfree[pb] = stage_out(mmp[pb], "copy", dst, t_mm)
                else:
                    half = blk % 2
                    for i in range(16):
                        pb = mmi % 4
                        mmi += 1
                        pe.wait(t_w, hT_ready, mmfree[pb])
                        for k in range(KC):
                            ins = pe.e.matmul(mmp[pb][:], lhsT=hT[:, k, i * P:(i + 1) * P], rhs=wr_[s][:, k, :],
                                              start=(k == 0), stop=(k == KC - 1))
                        t_mm = pe.mark(ins)
                        if blk in (2, 3):
                            ot = (xoff // P) + i
                            dst = gv_s[xoff + i * P: xoff + (i + 1) * P, half * 512:(half + 1) * 512]
                            mmfree[pb] = stage_out(mmp[pb], "gelu", dst, t_mm,
                                                   extra=(lnst[:, ot, half:half + 1], lnst[:, ot, 2 + half:3 + half]))
                        else:
                            dst = v_s[ext_off + i * P: ext_off + (i + 1) * P, half * 512:(half + 1) * 512]
                            mmfree[pb] = stage_out(mmp[pb], "copy", dst, t_mm)
                wfree[s] = pe.last()
                hT_readers = pe.last()
                if pend:
                    issue_wA(pend.pop(0))
        b.barrier()
    if STOP_AFTER == "A":
        return b, finish(b, out_d)

    class HeadNorm:
        def __init__(self, ph, tag, gcol):
            self.sq = sb("sq" + tag, [P, NH, 512], BF16, ph)
            self.msb = sb("msb" + tag, [P, 512], F32, ph)
            self.rsb = sb("rsb" + tag, [P, 512], F32, ph)
            self.ynT = [sb(f"ynT{tag}{i}", [P, NH, 512], BF16, ph) for i in range(2)]
            self.ssb = ps("ssb" + tag, [P, 512], F32, ph)
            self.yns = [Slot(b, f"yn{tag}_s{i}") for i in range(2)]
            self.gcol = gcol
            self.ynfree = [None, None]
            self.ssbfree = None
            self.sqfree = None
            self.rsfree = None
            self.n = 0

        def run(self, st, src, t_src, chunk0):
            act.wait(t_src, self.sqfree)
            t_sq = act.mark(act.e.activation(out=self.sq[:], in_=src, func=AF.Square))
            pe.wait(t_sq, self.ssbfree)
            for h in range(NH):
                ins = pe.e.matmul(self.ssb[:], lhsT=ones_b[:], rhs=self.sq[:, h, :], start=(h == 0), stop=(h == NH - 1))
            t_ss = pe.mark(ins)
            self.sqfree = t_ss
            act.wait(t_ss, self.rsfree, t_const)
            t_ln = act.mark(act.e.activation(out=self.rsb[:], in_=self.ssb[:], func=AF.Ln, bias=eps_t[:], scale=1.0 / DG))
            self.ssbfree = t_ln
            act.wait(t_ln)
            t_rs = act.mark(act.e.activation(out=self.rsb[:], in_=self.rsb[:], func=AF.Exp, scale=-0.5))
            s = self.n % 2
            self.n += 1
            dve.wait(t_rs, self.ynfree[s], t_src)
            for h in range(NH):
                ins = dve.e.scalar_tensor_tensor(out=self.ynT[s][:, h, :], in0=src[:, h, :],
                                                 scalar=self.gcol[:, chunk0 + h:chunk0 + h + 1], in1=self.rsb[:],
                                                 op0=ALU.mult, op1=ALU.mult)
            t_yn = dve.mark(ins)
            self.rsfree = t_yn
            sp.wait(t_yn)
            self.ynfree[s] = self.yns[s].dma(sp.e.dma_start(
                out=yT_s[chunk0:chunk0 + NH, :, st * 512:(st + 1) * 512].rearrange("h p t -> p h t"),
                in_=self.ynT[s][:]))
            return t_yn

    NSTB = TOWN // 512
    with ExitStack() as ph:
        gcol = sb("gcol", [P, KC], F32, ph)
        cB = Slot(b, "constB")
        t_gc = cB.dma(sp.e.dma_start(out=gcol[:], in_=gout_d))
        dve.wait(t_gc)

        with ExitStack() as p1:
            hn = HeadNorm(p1, "g", gcol)
            WsT = sb("WsT", [P, NH, P], BF16, p1)
            wtmp = sb("wtmp", [P, P], F32, p1)
            wmb = sb("wmb", [P, P], BF16, p1)
            tril_t = sb("tril_t", [P, P], F32, p1)
            bsr_f = sb("bsr_f", [1, NH * P], F32, p1)
            bsr = sb("bsr", [1, NH * P], BF16, p1)
            ggv_bc = sb("ggv_bc", [P, DG], F32, p1)
            mean = sb("meanB", [P, NT], F32, p1)
            ex2 = sb("ex2B", [P, NT], F32, p1)
            rstd = sb("rstdB", [P, NT], F32, p1)
            gvt = [sb(f"gvt{i}", [P, 4, DG], BF16, p1) for i in range(2)]
            uTt = [sb(f"uTt{i}", [P, NH, 512], BF16, p1) for i in range(2)]
            vtmp = sb("vtmp", [P, DG], F32, p1)
            vn = [sb(f"vn{i}", [P, DG], BF16, p1) for i in range(2)]
            yaT = [sb(f"yaT{i}", [P, NH, 512], BF16, p1) for i in range(2)]
            svp = [ps(f"svp{i}", [P, NH, P], F32, p1) for i in range(2)]
            tpw = ps("tpw", [P, P], BF16, p1)
            gvs = [Slot(b, f"gv_s{i}") for i in range(2)]
            uts = [Slot(b, f"ut_s{i}") for i in range(2)]

            zt = sb("zt", [P, 4, D], BF16, p1)
            zs = Slot(b, "zfill")
            t_z = dve.mark(dve.e.memset(zt[:], 0.0))
            t_tr = cB.dma(sp.e.dma_start(out=tril_t[:], in_=tril_d))
            t_bs = cB.dma(sp.e.dma_start(out=bsr_f[:], in_=bsp_d))
            t_gg = cB.dma(sp.e.dma_start(out=ggv_bc[:], in_=ggv_d.to_broadcast([P, DG])))
            dve.wait(t_bs)
            t_bsr = dve.mark(dve.e.tensor_copy(out=bsr[:], in_=bsr_f[:]))
            t_prev = None
            for h in range(NH):
                sp.wait(t_prev)
                t_w = cB.dma(sp.e.dma_start(out=wtmp[:], in_=wsp_d[h]))
                dve.wait(t_w, t_tr, t_prev)
                t_m = dve.mark(dve.e.tensor_tensor(out=wmb[:], in0=wtmp[:], in1=tril_t[:], op=ALU.mult))
                pe.wait(t_m, t_prev)
                t_t = pe.mark(pe.e.transpose(out=tpw[:], in_=wmb[:], identity=ident_b[:]))
                dve.wait(t_t)
                t_prev = dve.mark(dve.e.tensor_copy(out=WsT[:, h, :], in_=tpw[:]))
            t_wst = t_prev

            def dchain(ins):
                t = dve.mark(ins)
                dve.wait(t)
                return t
            dchain(dve.e.tensor_tensor(out=mean[:], in0=lnst[:, :, 0], in1=lnst[:, :, 1], op=ALU.add))
            dchain(dve.e.tensor_scalar(out=mean[:], in0=mean[:], scalar1=1.0 / DG, scalar2=None, op0=ALU.mult))
            dchain(dve.e.tensor_tensor(out=ex2[:], in0=lnst[:, :, 2], in1=lnst[:, :, 3], op=ALU.add))
            dchain(dve.e.tensor_scalar(out=ex2[:], in0=ex2[:], scalar1=1.0 / DG, scalar2=None, op0=ALU.mult))
            dchain(dve.e.tensor_tensor(out=rstd[:], in0=mean[:], in1=mean[:], op=ALU.mult))
            dchain(dve.e.tensor_tensor(out=ex2[:], in0=ex2[:], in1=rstd[:], op=ALU.subtract))
            t_var = dchain(dve.e.tensor_scalar(out=ex2[:], in0=ex2[:], scalar1=EPS, scalar2=None, op0=ALU.add))
            pool.wait(t_var)
            t_rstd = pool.mark(pool.e.tensor_tensor(out=rstd[:], in0=ex2[:], in1=neghalf[:, 0:NT], op=ALU.pow))

            gvfree = [None, None]
            utfree = [None, None]
            ld_tok = {}
            zfill_left = list(range(NE))

            def load_st(st):
                s = st % 2
                sp.wait(gvfree[s], utfree[s])
                tg = gvs[s].dma(sp.e.dma_start(
                    out=gvt[s][:], in_=gv_s[st * 512:(st + 1) * 512, :].rearrange("(j p) f -> p j f", p=P)))
                tu = uts[s].dma(sp.e.dma_start(
                    out=uTt[s][:], in_=uT_s[:, :, st * 512:(st + 1) * 512].rearrange("h p t -> p h t")))
                ld_tok[st] = (tg, tu)

            vtfree = None
            vnfree = [None, None]
            svfree = [None, None]
            yafree = [None, None]
            cnt = 0
            load_st(0)
            for st in range(NSTB):
                if st + 1 < NSTB:
                    load_st(st + 1)
                sp.wait(t_z)
                for _ in range(4):
                    e = zfill_left.pop(0)
                    zs.dma(sp.e.dma_start(out=xe_s[e * CAP:(e + 1) * CAP, :].rearrange("(b p) f -> p b f", p=P),
                                          in_=zt[:]))
                s = st % 2
                tg, tu = ld_tok[st]
                for j in range(4):
                    ot = st * 4 + j
                    c2 = cnt % 2
                    cnt += 1
                    dve.wait(tg, t_rstd, vtfree)
                    t_v1 = dve.mark(dve.e.tensor_scalar(out=vtmp[:], in0=gvt[s][:, j, :], scalar1=mean[:, ot:ot + 1],
                                                        scalar2=rstd[:, ot:ot + 1], op0=ALU.subtract, op1=ALU.mult))
                    pool.wait(t_v1, t_gg, vnfree[c2])
                    t_vn = pool.mark(pool.e.tensor_tensor(out=vn[c2][:], in0=vtmp[:], in1=ggv_bc[:], op=ALU.mult))
                    vtfree = t_vn
                    pe.wait(t_vn, t_wst, t_bsr, svfree[c2])
                    for h in range(NH):
                        pe.e.matmul(svp[c2][:, h, :], lhsT=vn[c2][:, h * P:(h + 1) * P], rhs=WsT[:, h, :],
                                    start=True, stop=False)
                        ins = pe.e.matmul(svp[c2][:, h, :], lhsT=ones_b[0:1, :], rhs=bsr[0:1, h * P:(h + 1) * P],
                                          start=False, stop=True)
                    t_sv = pe.mark(ins)
                    vnfree[c2] = t_sv
                    dve.wait(t_sv, tu)
                    if j == 0:
                        dve.wait(yafree[s])
                    for hh in range(2):
                        ins = dve.e.tensor_tensor(out=yaT[s][:, 4 * hh:4 * hh + 4, j * P:(j + 1) * P],
                                                  in0=svp[c2][:, 4 * hh:4 * hh + 4, :],
                                                  in1=uTt[s][:, 4 * hh:4 * hh + 4, j * P:(j + 1) * P], op=ALU.mult)
                    t_ya = dve.mark(ins)
                    svfree[c2] = t_ya
                gvfree[s] = t_v1
                utfree[s] = t_ya
                yafree[s] = hn.run(st, yaT[s][:], t_ya, 0)
            b.barrier()
        if STOP_AFTER == "B1":
            return b, finish(b, out_d)

        with ExitStack() as p2:
            maskf = sb("maskf", [P, 3, P], F32, p2)
            mpair = sb("mpair", [P, 2, 2, P], BF16, p2)
            qTt = [sb(f"qTt{i}", [P, 2048], BF16, p2) for i in range(2)]
            kTt = [sb(f"kTt{i}", [P, 4096], BF16, p2) for i in range(2)]
            NV = 10
            vpc = [sb(f"vpc{i}", [P, 2, P], BF16, p2) for i in range(NV)]
            NPT = 3
            pT = [sb(f"pT{i}", [P, 2, P], BF16, p2) for i in range(NPT)]
            acc2 = sb("acc2", [P, 2, 2048], F32, p2)
            rec = sb("rec", [P, 2048], F32, p2)
            ost = [sb(f"ost{i}", [P, 2048], BF16, p2) for i in range(2)]
            sps = [ps(f"sps{i}", [P, 2, P], F32, p2) for i in range(NPT)]
            olp = [ps(f"olp{i}", [P, 2, P], F32, p2) for i in range(2)]
            qs = [Slot(b, f"q_s{i}") for i in range(2)]
            ks = [Slot(b, f"k_s{i}") for i in range(2)]
            vs = [Slot(b, f"v_s{i}") for i in range(NV)]
            oss = [Slot(b, f"o_s{i}") for i in range(2)]

            mc2 = ModCalc(p2, "2")
            for j in range(NPC0, NPC0 + 3):
                mc2.issue(j)
            t_mf = cB.dma(sp.e.dma_start(out=maskf[:], in_=maskb_d))
            dve.wait(t_mf)
            dve.mark(dve.e.tensor_single_scalar(out=mpair[:, 0, 0, :], in_=maskf[:, 1, :], scalar=0.0, op=ALU.is_equal))
            dve.mark(dve.e.tensor_single_scalar(out=mpair[:, 0, 1, :], in_=maskf[:, 0, :], scalar=0.0, op=ALU.is_equal))
            dve.mark(dve.e.tensor_single_scalar(out=mpair[:, 1, 0, :], in_=maskf[:, 2, :], scalar=0.0, op=ALU.is_equal))
            t_mask = dve.mark(dve.e.tensor_single_scalar(out=mpair[:, 1, 1, :], in_=maskf[:, 0, :], scalar=0.0, op=ALU.is_equal))
            dve.wait(t_mask)

            units = [(h, spn) for h in range(NH) for spn in range(2)]
            qkfree = [None, None]
            vfree = [None] * NV
            pTfree = [None] * 3
            spsfree = [None] * 3
            olpfree = [None, None]
            ostfree = [None, None]
            acc_free = None
            scale = float(P) ** -0.5
            bi = 0
            qk_tok = {}

            def load_qk(u):
                h, spn = units[u]
                s = u % 2
                sp.wait(qkfree[s])
                tq = qs[s].dma(sp.e.dma_start(out=qTt[s][:], in_=qT_s[h, :, spn * 2048:(spn + 1) * 2048]))
                tk = ks[s].dma(sp.e.dma_start(out=kTt[s][:], in_=kT_s[h, :, spn * 2048:spn * 2048 + 4096]))
                qk_tok[u] = (tq, tk)

            load_qk(0)
            for u, (h, spn) in enumerate(units):
                if u + 1 < len(units):
                    load_qk(u + 1)
                s = u % 2
                tq, tk = qk_tok[u]
                blocks = []
                for d in (1, 4, 16):
                    Bt = P * d
                    for grp in range(2048 // Bt):
                        for r in range(d):
                            blocks.append((d, grp, r))
                nb = len(blocks)
                v_tok = {}
                s_tok = {}
                p_tok = {}
                copy_toks = []
                pat_done = {}

                def load_v(i):
                    d, grp, r = blocks[i]
                    Bt = P * d
                    q0 = grp * Bt + r
                    start = spn * 2048 + 2048 + q0 - Bt
                    vsl = (bi + i) % NV
                    sp.wait(vfree[vsl])
                    src = v_s[start:start + 255 * d + 1:d, h * P:(h + 1) * P].rearrange("(bb p) e -> p bb e", p=P)
                    v_tok[i] = (vsl, vs[vsl].dma(sp.e.dma_start(out=vpc[vsl][:], in_=src)))

                def emit_S(i):
                    d, grp, r = blocks[i]
                    Bt = P * d
                    q0 = grp * Bt + r
                    sb_ = (bi + i) % NPT
                    qsl = slice(q0, q0 + (P - 1) * d + 1, d)
                    kc0 = 2048 + q0
                    kp0 = kc0 - Bt
                    mi = 1 if (spn == 0 and grp == 0) else 0
                    pe.wait(tq, tk, spsfree[sb_])
                    pe.e.matmul(sps[sb_][:, 0, :], lhsT=kTt[s][:, kp0:kp0 + (P - 1) * d + 1:d], rhs=qTt[s][:, qsl],
                                start=True, stop=True)
                    ins = pe.e.matmul(sps[sb_][:, 1, :], lhsT=kTt[s][:, kc0:kc0 + (P - 1) * d + 1:d], rhs=qTt[s][:, qsl],
                                      start=True, stop=True)
                    s_tok[i] = pe.mark(ins)
                    act.wait(s_tok[i], pTfree[sb_])
                    t_e = act.mark(act.e.activation(out=pT[sb_][:], in_=sps[sb_][:], func=AF.Exp, scale=scale))
                    spsfree[sb_] = t_e
                    dve.wait(t_e, t_mask)
                    p_tok[i] = dve.mark(dve.e.tensor_tensor(out=pT[sb_][:], in0=pT[sb_][:], in1=mpair[:, mi], op=ALU.mult))

                def emit_PV(i):
                    nonlocal acc_free
                    d, grp, r = blocks[i]
                    Bt = P * d
                    q0 = grp * Bt + r
                    sb_ = (bi + i) % 2
                    pb_ = (bi + i) % NPT
                    vsl, tv = v_tok[i]
                    pe.wait(p_tok[i], tv, olpfree[sb_])
                    pe.e.matmul(olp[sb_][:, 0, :], lhsT=vpc[vsl][:, 0, :], rhs=pT[pb_][:, 0, :], start=True, stop=False)
                    pe.e.matmul(olp[sb_][:, 0, :], lhsT=vpc[vsl][:, 1, :], rhs=pT[pb_][:, 1, :], start=False, stop=True)
                    pe.e.matmul(olp[sb_][:, 1, :], lhsT=ones_b[:], rhs=pT[pb_][:, 0, :], start=True, stop=False)
                    ins = pe.e.matmul(olp[sb_][:, 1, :], lhsT=ones_b[:], rhs=pT[pb_][:, 1, :], start=False, stop=True)
                    t_ol = pe.mark(ins)
                    vfree[vsl] = t_ol
                    pTfree[pb_] = t_ol
                    dst = acc2[:, :, q0:q0 + (P - 1) * d + 1:d]
                    if d == 1:
                        act.wait(t_ol, acc_free)
                        t_acc = act.mark(act.e.copy(out=dst, in_=olp[sb_][:]))
                        copy_toks.append(t_acc)
                    else:
                        dve.wait(t_ol, acc_free, copy_toks, pat_done.get(d))
                        t_acc = dve.mark(dve.e.tensor_tensor(out=dst, in0=olp[sb_][:], in1=dst, op=ALU.add))
                        if d == 4:
                            pat_done[16] = t_acc
                    olpfree[sb_] = t_acc
                    return t_acc

                for i in range(min(NV - 1, nb)):
                    load_v(i)
                emit_S(0)
                emit_S(1)
                t_acc = None
                for i in range(nb):
                    if i + NV - 1 < nb:
                        load_v(i + NV - 1)
                    if i + 2 < nb:
                        emit_S(i + 2)
                    t_acc = emit_PV(i)
                bi += nb
                qkfree[s] = pe.last()
                act.wait(t_acc, acc_free)
                t_r0 = act.mark(act.e.activation(out=rec[:], in_=acc2[:, 1, :], func=AF.Ln))
                act.wait(t_r0)
                t_r = act.mark(act.e.activation(out=rec[:], in_=rec[:], func=AF.Exp, scale=-1.0))
                dve.wait(t_r, t_acc, ostfree[s])
                t_o = dve.mark(dve.e.tensor_tensor(out=ost[s][:], in0=acc2[:, 0, :], in1=rec[:], op=ALU.mult))
                acc_free = t_o
                sp.wait(t_o)
                ostfree[s] = oss[s].dma(sp.e.dma_start(out=yT_s[NH + h, :, spn * 2048:(spn + 1) * 2048], in_=ost[s][:]))
                jm = NPC0 + u
                if jm < NPC:
                    mc2.compute(jm)
                    if jm + 3 < NPC:
                        mc2.issue(jm + 3)
            b.barrier()
        if STOP_AFTER == "B2":
            return b, finish(b, out_d)

        with ExitStack() as p3:
            hn = HeadNorm(p3, "a", gcol)
            oTt = [sb(f"oTt{i}", [P, NH, 512], BF16, p3) for i in range(2)]
            ots = [Slot(b, f"ot_s{i}") for i in range(2)]
            ofree = [None, None]
            tl = {}

            def load_o(st):
                s = st % 2
                sp.wait(ofree[s])
                tl[st] = ots[s].dma(sp.e.dma_start(
                    out=oTt[s][:], in_=yT_s[NH:2 * NH, :, st * 512:(st + 1) * 512].rearrange("h p t -> p h t")))
            load_o(0)
            for st in range(NSTB):
                if st + 1 < NSTB:
                    load_o(st + 1)
                s = st % 2
                ofree[s] = hn.run(st, oTt[s][:], tl[st], NH)
            b.barrier()
    if STOP_AFTER == "B":
        return b, finish(b, out_d)

    with ExitStack() as ph:
        wo = sb("wo", [P, KC, D], BF16, ph)
        gg1 = sb("gg1_bc", [P, D], F32, ph)
        gm2 = sb("gm2_bc", [P, D], F32, ph)
        sh2 = sb("sh2_bc", [P, D], F32, ph)
        yTt = [sb(f"yTt{i}", [P, KC, 256], BF16, ph) for i in range(2)]
        xt = [sb(f"xtC{i}", [P, D], F32, ph) for i in range(2)]
        x1 = [sb(f"x1C{i}", [P, D], F32, ph) for i in range(2)]
        t1 = sb("t1C", [P, D], F32, ph)
        h2 = [sb(f"h2C{i}", [P, D], BF16, ph) for i in range(6)]
        junk = sb("junkC", [P, D], BF16, ph)
        h2T = sb("h2T", [P, KC, P], BF16, ph)
        wrb = sb("wrb", [P, KC, 36], BF16, ph)
        br_bc = sb("br_bc", [P, 36], F32, ph)
        ecap_bc = sb("ecap_bc", [P, NE], F32, ph)
        ustr_f = sb("ustr_f", [P, P], F32, ph)
        ustr_b = sb("ustr_b", [P, P], BF16, ph)
        base = sb("base", [P, NE], F32, ph)
        ssy = sb("ssy", [P, NT, 4], F32, ph)
        st_ = sb("statC", [P, NT, 6], F32, ph)
        rt = sb("rtC", [P, 64], F32, ph)
        L = sb("L", [P, 36], F32, ph)
        le = sb("le", [P, 8], F32, ph)
        le2 = sb("le2", [P, 8], F32, ph)
        oh = sb("oh", [P, 2, 8], F32, ph)
        goh = sb("goh", [P, 4], F32, ph)
        gexp = sb("gexp", [P, 4], F32, ph)
        E = sb("E", [P, 2, NE], F32, ph)
        E12 = sb("E12", [P, NE], BF16, ph)
        posm = sb("posm", [P, NE], F32, ph)
        vm = sb("vm", [P, NE], F32, ph)
        tmp32 = sb("tmp32", [P, NE], F32, ph)
        sidx = [sb(f"sidx{i}", [P, 2], I32, ph) for i in range(2)]
        y2p = [ps(f"y2p{i}", [P, 512], F32, ph) for i in range(4)]
        tpp = [ps(f"tpC{i}", [P, 4, P], BF16, ph) for i in range(2)]
        lgp = ps("lgp", [P, 4, 64], F32, ph)
        prp = ps("prp", [P, 5, NE], F32, ph)
        L4 = sb("L4", [P, 4, 36], F32, ph)
        r4 = sb("r4", [P, 14, 4], F32, ph)
        goh4 = sb("goh4", [P, 4, 4], F32, ph)
        gsh4 = sb("gsh4", [P, 4, 4], F32, ph)
        gex4 = sb("gex4", [P, 4, 4], F32, ph)
        le4 = sb("le4", [P, 4, 8], F32, ph)
        le24 = sb("le24", [P, 4, 8], F32, ph)
        tm8 = sb("tm8", [P, 4, 8], F32, ph)
        oh4 = sb("oh4", [P, 2, 4, 8], F32, ph)
        E4 = sb("E4", [P, 2, 4, NE], F32, ph)
        E124 = sb("E124", [P, 4, NE], BF16, ph)
        posm4 = sb("posm4", [P, 4, NE], F32, ph)
        vm4 = sb("vm4", [P, 4, NE], F32, ph)
        tm32 = sb("tm32", [P, 4, NE], F32, ph)
        w4 = sb("w4", [P, 2, 4], F32, ph)
        dbg4 = sb("dbg4", [P, 4, 4], F32, ph)
        sidx4 = [sb(f"sidx4{i}", [P, 4, 2], I32, ph) for i in range(2)]
        cC = Slot(b, "constC")
        wos = Slot(b, "wo_s")
        yts = [Slot(b, f"yt_s{i}") for i in range(2)]
        xs = [Slot(b, f"xC_s{i}") for i in range(2)]
        x1s = [Slot(b, f"x1C_s{i}") for i in range(2)]
        scs = [Slot(b, f"sc_s{i}") for i in range(2)]

        t_wo = None
        for n in range(4):
            t_wo = wos.dma(pool.e.dma_start(out=wo[:, :, n * 512:(n + 1) * 512],
                                            in_=wout_d[:, n * 512:(n + 1) * 512].rearrange("(k p) f -> p k f", p=P)))
        t_wr = wos.dma(pool.e.dma_start(out=wrb[:], in_=wr_d.rearrange("(k p) f -> p k f", p=P)))
        t_c = [cC.dma(sp.e.dma_start(out=gg1[:], in_=gpost1_d.to_broadcast([P, D]))),
               cC.dma(sp.e.dma_start(out=t1[:], in_=mod_s[0:1, 2 * D:3 * D].to_broadcast([P, D])))]
        dve.wait(*t_c)
        t_gg1 = dve.mark(dve.e.tensor_tensor(out=gg1[:], in0=gg1[:], in1=t1[:], op=ALU.mult))
        sp.wait(t_gg1)
        t_c = [cC.dma(sp.e.dma_start(out=gm2[:], in_=gpre2_d.to_broadcast([P, D]))),
               cC.dma(sp.e.dma_start(out=t1[:], in_=mod_s[0:1, 4 * D:5 * D].to_broadcast([P, D]))),
               cC.dma(sp.e.dma_start(out=sh2[:], in_=mod_s[0:1, 3 * D:4 * D].to_broadcast([P, D]))),
               cC.dma(sp.e.dma_start(out=br_bc[:], in_=br_d.to_broadcast([P, 36]))),
               cC.dma(sp.e.dma_start(out=ecap_bc[:], in_=ecap_d.to_broadcast([P, NE]))),
               cC.dma(sp.e.dma_start(out=ustr_f[:], in_=ustr_d))]
        dve.wait(*t_c)
        dve.mark(dve.e.scalar_tensor_tensor(out=gm2[:], in0=t1[:], scalar=1.0, in1=gm2[:], op0=ALU.add, op1=ALU.mult))
        dve.mark(dve.e.tensor_copy(out=ustr_b[:], in_=ustr_f[:]))
        dve.mark(dve.e.memset(base[:], 0.0))
        dve.mark(dve.e.memset(ssy[:], 0.0))
        t_setup = dve.mark(dve.e.memset(st_[:], 0.0))
        dve.wait(t_setup)

        def dch(ins):
            t = dve.mark(ins)
            dve.wait(t)
            return t

        ytfree = [None, None]
        yt_tok = {}

        def load_yt(st):
            s = st % 2
            sp.wait(ytfree[s])
            yt_tok[st] = yts[s].dma(sp.e.dma_start(
                out=yTt[s][:], in_=yT_s[:, :, st * 256:(st + 1) * 256].rearrange("c p t -> p c t")))

        xfree = [None, None]
        x_tok = {}

        def load_x(t):
            s = t % 2
            sp.wait(xfree[s])
            x_tok[t] = xs[s].dma(sp.e.dma_start(out=xt[s][:], in_=x_d[t * P:(t + 1) * P, :]))

        y2s = [sb(f"y2s{i}", [P, D], F32, ph) for i in range(2)]
        y2free = [None] * 4
        y2sfree = [None, None]
        t1free = t_setup
        x1free = [None, None]
        h2free = [None] * 6
        tpfree = [None, None]
        h2Tfree = None
        lgfree = None
        prfree = None
        sidxfree = [None, None]
        tokA = {}
        tokB = {}

        def st_a(t):
            st, j = t // 2, t % 2
            if j == 0 and st + 1 < NT // 2:
                load_yt(st + 1)
            s = t % 2
            ys = st % 2
            t_y2 = []
            for n in range(4):
                pe.wait(yt_tok[st], t_wo, y2free[n])
                for k in range(KC):
                    ins = pe.e.matmul(y2p[n][:], lhsT=yTt[ys][:, k, j * P:(j + 1) * P], rhs=wo[:, k, n * 512:(n + 1) * 512],
                                      start=(k == 0), stop=(k == KC - 1))
                t_y2.append(pe.mark(ins))
            if j == 1:
                ytfree[ys] = t_y2[-1]
            for n in range(4):
                act.wait(t_y2[n], t_setup, y2sfree[s])
                act.mark(act.e.activation(out=junk[:, n * 512:(n + 1) * 512], in_=y2p[n][:], func=AF.Square,
                                          accum_out=ssy[:, t, n:n + 1]))
                y2free[n] = act.mark(act.e.copy(out=y2s[s][:, n * 512:(n + 1) * 512], in_=y2p[n][:]))
            tokA[t] = act.last()

        tokB1 = {}

        tokB1a = {}
        tokB1b = {}

        def st_b1a(t):
            dve.wait(tokA[t])
            t_s = dch(dve.e.reduce_sum(out=st_[:, t, 0:1], in_=ssy[:, t, :], axis=AX.X))
            tokB1a[t] = rsqrt_small(st_[:, t, 0:1], st_[:, t, 1:2], st_[:, t, 2:3], 1.0 / D, 1, t_s)

        def st_b1b(t):
            s = t % 2
            dve.wait(tokB1a[t], t_gg1, x1free[s])
            t_t1 = dve.mark(dve.e.scalar_tensor_tensor(out=x1[s][:], in0=y2s[s][:], scalar=st_[:, t, 2:3], in1=gg1[:],
                                                       op0=ALU.mult, op1=ALU.mult))
            y2sfree[s] = t_t1
            dve.wait(t_t1, x_tok[t])
            t_x1 = dve.mark(dve.e.tensor_tensor(out=x1[s][:], in0=x1[s][:], in1=xt[s][:], op=ALU.add))
            xfree[s] = t_x1
            sp.wait(t_x1)
            t_x1st = x1s[s].dma(sp.e.dma_start(out=x1_s[t * P:(t + 1) * P, :], in_=x1[s][:]))
            if t + 2 < NT:
                load_x(t + 2)
            act.wait(t_x1)
            t_sq2 = act.mark(act.e.activation(out=junk[:], in_=x1[s][:], func=AF.Square, accum_out=st_[:, t, 3:4]))
            tokB1b[t] = (t_sq2, t_x1st)

        def st_b1c(t):
            t_sq2, t_x1st = tokB1b[t]
            t_rs2 = rsqrt_small(st_[:, t, 3:4], st_[:, t, 4:5], st_[:, t, 5:6], 1.0 / D, 1, t_sq2)
            tokB1[t] = (t_rs2, t_x1st)

        def st_b2(t):
            nonlocal t1free
            s = t % 2
            t_rs2, t_x1st = tokB1[t]
            dve.wait(t_rs2, t1free)
            t_t1b = dve.mark(dve.e.scalar_tensor_tensor(out=t1[:], in0=x1[s][:], scalar=st_[:, t, 5:6], in1=gm2[:],
                                                        op0=ALU.mult, op1=ALU.mult))
            x1free[s] = [t_t1b, t_x1st]
            pool.wait(t_t1b, h2free[t % 6])
            t_h2 = pool.mark(pool.e.tensor_tensor(out=h2[t % 6][:], in0=t1[:], in1=sh2[:], op=ALU.add))
            t1free = t_h2
            tokB[t] = t_h2

        NG = NT // 4
        H2R = 6
        lg_tok = {}
        h2_tok = {}
        tp_tok = {}

        def st_c1(t):
            nonlocal h2Tfree
            s = t % H2R
            j = t % 4
            t_h2 = tokB[t]
            t_evs = []
            for g in range(4):
                pb = g % 2
                pe.wait(t_h2, tpfree[pb])
                for jj in range(4):
                    k = 4 * g + jj
                    ins = pe.e.transpose(out=tpp[pb][:, jj, :], in_=h2[s][:, k * P:(k + 1) * P], identity=ident_b[:])
                t_tp = pe.mark(ins)
                ev = act if g % 2 == 0 else dve
                ev.wait(t_tp, h2Tfree)
                if ev is act:
                    t_ev = act.mark(act.e.copy(out=h2T[:, 4 * g:4 * g + 4, :], in_=tpp[pb][:]))
                else:
                    t_ev = dve.mark(dve.e.tensor_copy(out=h2T[:, 4 * g:4 * g + 4, :], in_=tpp[pb][:]))
                tpfree[pb] = t_ev
                t_evs.append(t_ev)
            pe.wait(t_evs, t_wr)
            if j == 0:
                pe.wait(lgfree)
            for k in range(KC):
                ins = pe.e.matmul(lgp[:, j, 0:36], lhsT=h2T[:, k, :], rhs=wrb[:, k, :], start=(k == 0), stop=(k == KC - 1))
            t_lg = pe.mark(ins)
            h2Tfree = t_lg
            lg_tok[t] = t_lg
            h2_tok[t] = t_h2
            tp_tok[t] = t_tp

        def bc(ap2, n):
            return ap2[:, :, None].to_broadcast([P, 4, n])

        def st_c2(g):
            nonlocal lgfree, prfree
            sg_ = g % 2
            dve.wait(lg_tok[4 * g + 3])
            t_L = dch(dve.e.tensor_tensor(out=L4[:], in0=lgp[:, :, 0:36], in1=br_bc[:, None, :].to_broadcast([P, 4, 36]),
                                          op=ALU.add))
            lgfree = t_L
            dch(dve.e.reduce_max(out=r4[:, 0, :], in_=L4[:, :, 0:4], axis=AX.X))
            dch(dve.e.tensor_tensor(out=goh4[:], in0=L4[:, :, 0:4], in1=bc(r4[:, 0, :], 4), op=ALU.is_equal))
            t_gs = dch(dve.e.tensor_tensor(out=gsh4[:], in0=L4[:, :, 0:4], in1=bc(r4[:, 0, :], 4), op=ALU.subtract))
            act.wait(t_gs)
            t_ge = act.mark(act.e.activation(out=gex4[:], in_=gsh4[:], func=AF.Exp))
            dch(dve.e.tensor_tensor(out=le4[:], in0=L4[:, :, 4:12], in1=bc(goh4[:, :, 0], 8), op=ALU.mult))
            for gg in range(1, 4):
                dch(dve.e.tensor_tensor(out=tm8[:], in0=L4[:, :, 4 + 8 * gg:12 + 8 * gg], in1=bc(goh4[:, :, gg], 8), op=ALU.mult))
                dch(dve.e.tensor_tensor(out=le4[:], in0=le4[:], in1=tm8[:], op=ALU.add))
            dch(dve.e.reduce_max(out=r4[:, 1, :], in_=le4[:], axis=AX.X))
            dch(dve.e.tensor_tensor(out=oh4[:, 0], in0=le4[:], in1=bc(r4[:, 1, :], 8), op=ALU.is_equal))
            dch(dve.e.scalar_tensor_tensor(out=le24[:], in0=oh4[:, 0], scalar=-1e30, in1=le4[:], op0=ALU.mult, op1=ALU.add))
            dch(dve.e.reduce_max(out=r4[:, 2, :], in_=le24[:], axis=AX.X))
            dch(dve.e.tensor_tensor(out=oh4[:, 1], in0=le24[:], in1=bc(r4[:, 2, :], 8), op=ALU.is_equal))
            t_dd = dch(dve.e.tensor_tensor(out=r4[:, 3, :], in0=r4[:, 2, :], in1=r4[:, 1, :], op=ALU.subtract))
            act.wait(t_dd)
            t_ed = act.mark(act.e.activation(out=r4[:, 4, :], in_=r4[:, 3, :], func=AF.Exp))
            for kk in range(2):
                for gg in range(4):
                    ins = dve.e.tensor_tensor(out=E4[:, kk, :, 8 * gg:8 * gg + 8], in0=oh4[:, kk], in1=bc(goh4[:, :, gg], 8),
                                              op=ALU.mult)
            dch(ins)
            dve.wait(prfree)
            t_e12 = dch(dve.e.tensor_tensor(out=E124[:], in0=E4[:, 0], in1=E4[:, 1], op=ALU.add))
            pe.wait(t_e12, t_setup)
            for j in range(4):
                for jp in range(j):
                    pe.e.matmul(prp[:, j, :], lhsT=ones_b[:], rhs=E124[:, jp, :], start=(jp == 0), stop=False)
                pe.e.matmul(prp[:, j, :], lhsT=ustr_b[:], rhs=E124[:, j, :], start=(j == 0), stop=True)
            for j in range(4):
                ins = pe.e.matmul(prp[:, 4, :], lhsT=ones_b[:], rhs=E124[:, j, :], start=(j == 0), stop=(j == 3))
            t_pr = pe.mark(ins)
            dve.wait(t_ge, t_ed)
            dch(dve.e.reduce_sum(out=r4[:, 5, :], in_=gex4[:], axis=AX.X))
            dch(dve.e.reciprocal(out=r4[:, 6, :], in_=r4[:, 5, :]))
            dch(dve.e.tensor_scalar(out=r4[:, 7, :], in0=r4[:, 4, :], scalar1=1.0, scalar2=None, op0=ALU.add))
            dch(dve.e.reciprocal(out=r4[:, 8, :], in_=r4[:, 7, :]))
            dch(dve.e.tensor_tensor(out=w4[:, 0, :], in0=r4[:, 8, :], in1=r4[:, 6, :], op=ALU.mult))
            dch(dve.e.tensor_tensor(out=w4[:, 1, :], in0=r4[:, 4, :], in1=w4[:, 0, :], op=ALU.mult))
            dve.wait(t_pr)
            dch(dve.e.tensor_tensor(out=posm4[:], in0=prp[:, 0:4, :], in1=base[:, None, :].to_broadcast([P, 4, NE]), op=ALU.add))
            t_base = dch(dve.e.tensor_tensor(out=base[:], in0=prp[:, 4, :], in1=base[:], op=ALU.add))
            prfree = t_base
            dch(dve.e.tensor_single_scalar(out=vm4[:], in_=posm4[:], scalar=float(CAP), op=ALU.is_lt))
            dch(dve.e.tensor_tensor(out=posm4[:], in0=posm4[:], in1=ecap_bc[:, None, :].to_broadcast([P, 4, NE]), op=ALU.add))
            dve.wait(sidxfree[sg_])
            for kk in range(2):
                dch(dve.e.tensor_tensor(out=tm32[:], in0=E4[:, kk], in1=posm4[:], op=ALU.mult))
                dch(dve.e.reduce_sum(out=r4[:, 9, :], in_=tm32[:], axis=AX.X))
                dch(dve.e.tensor_tensor(out=tm32[:], in0=E4[:, kk], in1=vm4[:], op=ALU.mult))
                dch(dve.e.reduce_sum(out=r4[:, 10, :], in_=tm32[:], axis=AX.X))
                dch(dve.e.tensor_tensor(out=r4[:, 11, :], in0=r4[:, 9, :], in1=r4[:, 10, :], op=ALU.mult))
                dch(dve.e.tensor_copy(out=ridx[:, 4 * g:4 * g + 4, kk], in_=r4[:, 11, :]))
                dch(dve.e.tensor_tensor(out=rwt[:, 4 * g:4 * g + 4, kk], in0=w4[:, kk, :], in1=r4[:, 10, :], op=ALU.mult))
                dch(dve.e.tensor_scalar(out=r4[:, 12, :], in0=r4[:, 10, :], scalar1=-float(NSLOT), scalar2=float(NSLOT),
                                        op0=ALU.mult, op1=ALU.add))
                dch(dve.e.tensor_tensor(out=r4[:, 13, :], in0=r4[:, 12, :], in1=r4[:, 11, :], op=ALU.add))
                t_si = dch(dve.e.tensor_copy(out=sidx4[sg_][:, :, kk], in_=r4[:, 13, :]))
                if DEBUG:
                    dch(dve.e.tensor_copy(out=dbg4[:, :, 2 + kk], in_=r4[:, 9, :]))
                    dch(dve.e.tensor_copy(out=dbg4[:, :, kk], in_=w4[:, kk, :]))
            for j in range(4):
                t = 4 * g + j
                pool.wait(t_si, h2_tok[t])
                for kk in range(2):
                    t_sc = scs[sg_].dma(pool.e.indirect_dma_start(
                        out=xe_s, out_offset=bass.IndirectOffsetOnAxis(ap=sidx4[sg_][:, j, kk:kk + 1], axis=0),
                        in_=h2[t % H2R][:], in_offset=None))
                h2free[t % H2R] = [t_sc, tp_tok[t]]
            sidxfree[sg_] = t_sc
            if DEBUG:
                sp.wait(dve.last())
                cC.dma(sp.e.dma_start(out=rt_s[:, g * 16:(g + 1) * 16], in_=dbg4[:].rearrange("p j c -> p (j c)")))
                dve.wait(cC.last())

        load_yt(0)
        load_x(0)
        load_x(1)
        for i in range(NT + 3):
            if i < NT:
                st_a(i)
            if 0 <= i - 1 < NT:
                st_b1a(i - 1)
            if 0 <= i - 2 < NT:
                st_b2(i - 2)
            if 0 <= i - 3 < NT:
                st_c1(i - 3)
            if 0 <= i - 1 < NT:
                st_b1b(i - 1)
            if 0 <= i - 3 < NT and (i - 3) % 4 == 3:
                st_c2((i - 3) // 4)
            if 0 <= i - 1 < NT:
                st_b1c(i - 1)
        dve.wait(dve.last())
        dve.mark(dve.e.tensor_copy(out=cnt_i[:], in_=base[:]))
        b.barrier()
    if STOP_AFTER == "D":
        return b, finish(b, out_d)

    with ExitStack() as ph:
        NBE = 5
        wE = [sb(f"wE{i}", [P, 16 * 512], BF16, ph) for i in range(NBE)]
        xe = [sb(f"xe{i}", [P, 4, D], BF16, ph) for i in range(2)]
        xeT = [sb(f"xeT{i}", [P, KC, CAP], BF16, ph) for i in range(2)]
        actT = sb("actT", [P, 8, CAP], BF16, ph)
        sg = [sb(f"sg{i}", [P, CAP], BF16, ph) for i in range(2)]
        yst = [sb(f"yst{i}", [P, 1024], BF16, ph) for i in range(2)]
        tpp = [ps(f"tpE{i}", [P, 4, P], BF16, ph) for i in range(2)]
        gup = [ps(f"gup{i}", [P, 512], F32, ph) for i in range(4)]
        dnp = [ps(f"dnp{i}", [P, 512], F32, ph) for i in range(2)]
        wEs = [Slot(b, f"wE_s{i}") for i in range(NBE)]
        xes = [Slot(b, f"xe_s{i}") for i in range(2)]
        yss = [Slot(b, f"ys_s{i}") for i in range(2)]

        pieces = []
        for e in range(NE):
            for hf in range(2):
                pieces.append((e, "g", hf))
                pieces.append((e, "u", hf))
            for hf in range(2):
                pieces.append((e, "d", hf))
        wfree = [None] * NBE
        w_tok = {}
        wi = 0

        def issue_piece():
            nonlocal wi
            if wi >= len(pieces):
                return
            e, kind, hf = pieces[wi]
            s = wi % NBE
            pool.wait(wfree[s])
            if kind == "g":
                src = wg_d[e, :, hf * 512:(hf + 1) * 512].rearrange("(k p) f -> p k f", p=P)
                dst = wE[s][:].rearrange("p (k f) -> p k f", k=KC)
            elif kind == "u":
                src = wu_d[e, :, hf * 512:(hf + 1) * 512].rearrange("(k p) f -> p k f", p=P)
                dst = wE[s][:].rearrange("p (k f) -> p k f", k=KC)
            else:
                src = wd_d[e, :, hf * 1024:(hf + 1) * 1024].rearrange("(k p) f -> p k f", p=P)
                dst = wE[s][:].rearrange("p (k f) -> p k f", k=8)
            w_tok[(e, kind, hf)] = (s, wEs[s].dma(pool.e.dma_start(out=dst, in_=src)))
            wi += 1

        def issue_piece_st(ST):
            if ST["wi"] >= len(pieces):
                return
            e_, kind, hf = pieces[ST["wi"]]
            s_ = ST["wi"] % NBE
            pool.wait(ST["wfree"][s_])
            if kind == "g":
                src = wg_d[e_, :, hf * 512:(hf + 1) * 512].rearrange("(k p) f -> p k f", p=P)
                dst = wE[s_][:].rearrange("p (k f) -> p k f", k=KC)
            elif kind == "u":
                src = wu_d[e_, :, hf * 512:(hf + 1) * 512].rearrange("(k p) f -> p k f", p=P)
                dst = wE[s_][:].rearrange("p (k f) -> p k f", k=KC)
            else:
                src = wd_d[e_, :, hf * 1024:(hf + 1) * 1024].rearrange("(k p) f -> p k f", p=P)
                dst = wE[s_][:].rearrange("p (k f) -> p k f", k=8)
            ST["w_tok"][(e_, kind, hf)] = (s_, wEs[s_].dma(pool.e.dma_start(out=dst, in_=src)))
            ST["wi"] += 1

        cnt_reg = {x.name: es.enter_context(x.real.register("cnt_" + x.name)) for x in (pe, act, dve)}

        xefree = [None, None]
        xe_tok = {}

        def load_xe(e):
            s = e % 2
            sp.wait(xefree[s])
            xe_tok[e] = xes[s].dma(sp.e.dma_start(
                out=xe[s][:], in_=xe_s[e * CAP:(e + 1) * CAP, :].rearrange("(bb p) f -> p bb f", p=P)))

        for _ in range(NBE):
            issue_piece()
        load_xe(0)
        tpfree = [None, None]
        xeTfree = [None, None]
        gupfree = [None] * 4
        dnpfree = [None, None]
        sgfree = [None, None]
        actfree = None
        ystfree = [None, None]
        gi = 0
        di = 0
        tpi = 0
        for e in range(NE):
            if e + 1 < NE:
                load_xe(e + 1)
            s = e % 2
            t_xT = []
            for bb in range(4):
                for g in range(4):
                    pb = tpi % 2
                    tpi += 1
                    pe.wait(xe_tok[e], tpfree[pb])
                    for jj in range(4):
                        k = 4 * g + jj
                        ins = pe.e.transpose(out=tpp[pb][:, jj, :], in_=xe[s][:, bb, k * P:(k + 1) * P], identity=ident_b[:])
                    t_tp = pe.mark(ins)
                    ev = act if (tpi % 2 == 0) else dve
                    ev.wait(t_tp, xeTfree[s])
                    if ev is act:
                        t_ev = act.mark(act.e.copy(out=xeT[s][:, 4 * g:4 * g + 4, bb * P:(bb + 1) * P], in_=tpp[pb][:]))
                    else:
                        t_ev = dve.mark(dve.e.tensor_copy(out=xeT[s][:, 4 * g:4 * g + 4, bb * P:(bb + 1) * P], in_=tpp[pb][:]))
                    tpfree[pb] = t_ev
                    t_xT.append(t_ev)
            xefree[s] = t_tp
            ST = dict(gi=gi, gupfree=list(gupfree), sgfree=list(sgfree), wfree=list(wfree), wi=wi,
                      w_tok=dict(w_tok), t_act_all=[])

            def gate_up(nblk, ST):
                N = nblk * P
                for hf in range(2):
                    sgw, t_gw = ST["w_tok"][(e, "g", hf)]
                    suw, t_uw = ST["w_tok"][(e, "u", hf)]
                    gv_ = wE[sgw][:].rearrange("p (k f) -> p k f", k=KC)
                    uv_ = wE[suw][:].rearrange("p (k f) -> p k f", k=KC)
                    for m in range(4):
                        j = hf * 4 + m
                        pg = ST["gi"] % 4
                        pu = (ST["gi"] + 1) % 4
                        ST["gi"] += 2
                        pe.wait(t_gw, t_uw, t_xT, ST["gupfree"][pg], ST["gupfree"][pu])
                        for k in range(KC):
                            ins = pe.e.matmul(gup[pg][:, 0:N], lhsT=gv_[:, k, m * P:(m + 1) * P], rhs=xeT[s][:, k, 0:N],
                                              start=(k == 0), stop=(k == KC - 1))
                        t_g = pe.mark(ins)
                        for k in range(KC):
                            ins = pe.e.matmul(gup[pu][:, 0:N], lhsT=uv_[:, k, m * P:(m + 1) * P], rhs=xeT[s][:, k, 0:N],
                                              start=(k == 0), stop=(k == KC - 1))
                        t_u = pe.mark(ins)
                        ss_ = j % 2
                        act.wait(t_g, ST["sgfree"][ss_])
                        t_sg = act.mark(act.e.activation(out=sg[ss_][:, 0:N], in_=gup[pg][:, 0:N], func=AF.Silu))
                        ST["gupfree"][pg] = t_sg
                        dve.wait(t_sg, t_u)
                        if j == 0:
                            dve.wait(actfree)
                        t_a = dve.mark(dve.e.tensor_tensor(out=actT[:, j, 0:N], in0=gup[pu][:, 0:N], in1=sg[ss_][:, 0:N],
                                                           op=ALU.mult))
                        ST["gupfree"][pu] = t_a
                        ST["sgfree"][ss_] = t_a
                        ST["t_act_all"].append(t_a)
                    ST["wfree"][sgw] = pe.last()
                    ST["wfree"][suw] = pe.last()
                    for _ in range(2):
                        issue_piece_st(ST)

            def snap_all():
                return ([(x.n, dict(x.seen)) for x in b.engs], [sl.n for sl in b.slots])

            def restore_all(sn):
                for x, (n_, seen_) in zip(b.engs, sn[0]):
                    x.n = n_
                    x.seen = dict(seen_)
                for sl, n_ in zip(b.slots, sn[1]):
                    sl.n = n_

            def copy_ST(S0):
                return dict(gi=S0["gi"], gupfree=list(S0["gupfree"]), sgfree=list(S0["sgfree"]), wfree=list(S0["wfree"]),
                            wi=S0["wi"], w_tok=dict(S0["w_tok"]), t_act_all=[])

            sn0 = snap_all()
            ST_end = None
            for y_ in b.engs:
                y_.muted = True
            pool.muted = False
            restore_all(sn0)
            ST_end = copy_ST(ST)
            gate_up(4, ST_end)
            for X in (pe, act, dve):
                for y_ in b.engs:
                    y_.muted = True
                X.muted = False
                X.real.reg_load(cnt_reg[X.name], cnt_i[0:1, e:e + 1])
                with X.real.If_lt(cnt_reg[X.name], 2 * P + 1):
                    restore_all(sn0)
                    ST_end = copy_ST(ST)
                    gate_up(2, ST_end)
                with X.real.Else():
                    with X.real.If_lt(cnt_reg[X.name], 3 * P + 1):
                        restore_all(sn0)
                        ST_end = copy_ST(ST)
                        gate_up(3, ST_end)
                    with X.real.Else():
                        restore_all(sn0)
                        ST_end = copy_ST(ST)
                        gate_up(4, ST_end)
            for y_ in b.engs:
                y_.muted = False
            gi = ST_end["gi"]
            gupfree = ST_end["gupfree"]
            sgfree = ST_end["sgfree"]
            wfree = ST_end["wfree"]
            wi = ST_end["wi"]
            w_tok = ST_end["w_tok"]
            t_act_all = ST_end["t_act_all"]
            xeTfree[s] = pe.last()
            for hf in range(2):
                sdw, t_dw = w_tok[(e, "d", hf)]
                dv_ = wE[sdw][:].rearrange("p (k f) -> p k f", k=8)
                for bb in range(4):
                    ys_ = di % 2
                    di += 1
                    t_ev = None
                    for n2 in range(2):
                        pd = (2 * di + n2) % 2
                        pe.wait(t_dw, t_act_all, dnpfree[pd])
                        for jk in range(8):
                            ins = pe.e.matmul(dnp[pd][:], lhsT=actT[:, jk, bb * P:(bb + 1) * P],
                                              rhs=dv_[:, jk, n2 * 512:(n2 + 1) * 512], start=(jk == 0), stop=(jk == 7))
                        t_d = pe.mark(ins)
                        ev = act if n2 == 0 else dve
                        ev.wait(t_d, ystfree[ys_])
                        if ev is act:
                            t_e = act.mark(act.e.copy(out=yst[ys_][:, n2 * 512:(n2 + 1) * 512], in_=dnp[pd][:]))
                        else:
                            t_e = dve.mark(dve.e.tensor_copy(out=yst[ys_][:, n2 * 512:(n2 + 1) * 512], in_=dnp[pd][:]))
                        dnpfree[pd] = t_e
                        sp.wait(t_e)
                    r0 = e * CAP + bb * P
                    ystfree[ys_] = yss[ys_].dma(sp.e.dma_start(out=ye_s[r0:r0 + P, hf * 1024:(hf + 1) * 1024], in_=yst[ys_][:]))
                wfree[sdw] = pe.last()
                issue_piece()
            actfree = pe.last()
        b.barrier()
    if STOP_AFTER == "E":
        return b, finish(b, out_d)

    with ExitStack() as ph:
        gg2 = sb("gg2_bc", [P, D], F32, ph)
        tF = sb("tF", [P, D], F32, ph)
        Y = [[sb(f"Y{i}{k}", [P, D], BF16, ph) for k in range(2)] for i in range(2)]
        x1t = [sb(f"x1F{i}", [P, D], F32, ph) for i in range(2)]
        ym = [sb(f"ymF{i}", [P, D], F32, ph) for i in range(2)]
        ot = [sb(f"otF{i}", [P, D], F32, ph) for i in range(2)]
        junk = sb("junkF", [P, D], BF16, ph)
        stF = sb("stF", [P, NT, 3], F32, ph)
        cF = Slot(b, "constF")
        gs = [[Slot(b, f"gF_s{i}{k}") for k in range(2)] for i in range(2)]
        x1s = [Slot(b, f"x1F_s{i}") for i in range(2)]
        os_ = [Slot(b, f"oF_s{i}") for i in range(2)]
        t_c = [cF.dma(sp.e.dma_start(out=gg2[:], in_=gpost2_d.to_broadcast([P, D]))),
               cF.dma(sp.e.dma_start(out=tF[:], in_=mod_s[0:1, 5 * D:6 * D].to_broadcast([P, D])))]
        dve.wait(*t_c)
        t_gg2 = dve.mark(dve.e.tensor_tensor(out=gg2[:], in0=gg2[:], in1=tF[:], op=ALU.mult))
        t_z = dve.mark(dve.e.memset(stF[:], 0.0))
        Yfree = [None, None]
        x1free = [None, None]
        ymfree = [None, None]
        otfree = [None, None]
        tFfree = t_gg2
        ld = {}

        def loadY(t):
            s = t % 2
            pool.wait(Yfree[s])
            ld[t] = [gs[s][k].dma(pool.e.indirect_dma_start(
                out=Y[s][k][:], out_offset=None, in_=ye_s,
                in_offset=bass.IndirectOffsetOnAxis(ap=ridx[:, t, k:k + 1], axis=0))) for k in range(2)]

        ldx = {}

        def loadX(t):
            s = t % 2
            sp.wait(x1free[s])
            ldx[t] = x1s[s].dma(sp.e.dma_start(out=x1t[s][:], in_=x1_s[t * P:(t + 1) * P, :]))

        tokF = {}

        def stF1(t):
            s = t % 2
            tg = ld[t]
            act.wait(tg[0], ymfree[s])
            t_a = act.mark(act.e.activation(out=ym[s][:], in_=Y[s][0][:], func=AF.Identity, scale=rwt[:, t, 0:1]))
            dve.wait(t_a, tg)
            t_b = dve.mark(dve.e.scalar_tensor_tensor(out=ym[s][:], in0=Y[s][1][:], scalar=rwt[:, t, 1:2], in1=ym[s][:],
                                                      op0=ALU.mult, op1=ALU.add))
            Yfree[s] = t_b
            if t + 2 < NT:
                loadY(t + 2)
            act.wait(t_b, t_z)
            tokSq[t] = act.mark(act.e.activation(out=junk[:], in_=ym[s][:], func=AF.Square, accum_out=stF[:, t, 0:1]))

        tokSq = {}

        def stF1b(t):
            tokF[t] = rsqrt_small(stF[:, t, 0:1], stF[:, t, 1:2], stF[:, t, 2:3], 1.0 / D, 1, tokSq[t])

        def stF2(t):
            nonlocal tFfree
            s = t % 2
            tx = ldx[t]
            dve.wait(tokF[t], tFfree)
            t_t = dve.mark(dve.e.scalar_tensor_tensor(out=tF[:], in0=ym[s][:], scalar=stF[:, t, 2:3], in1=gg2[:],
                                                      op0=ALU.mult, op1=ALU.mult))
            ymfree[s] = t_t
            HF = D // 2
            pool.wait(t_t, tx, otfree[s])
            t_o1 = pool.mark(pool.e.tensor_tensor(out=ot[s][:, 0:HF], in0=tF[:, 0:HF], in1=x1t[s][:, 0:HF], op=ALU.add))
            dve.wait(t_t, tx, otfree[s])
            t_o2 = dve.mark(dve.e.tensor_tensor(out=ot[s][:, HF:D], in0=tF[:, HF:D], in1=x1t[s][:, HF:D], op=ALU.add))
            t_o = [t_o1, t_o2]
            tFfree = t_o
            x1free[s] = t_o
            sp.wait(t_o)
            otfree[s] = os_[s].dma(sp.e.dma_start(out=out_d[t * P:(t + 1) * P, :], in_=ot[s][:]))
            if t + 2 < NT:
                loadX(t + 2)

        loadY(0)
        loadY(1)
        loadX(0)
        loadX(1)
        for i in range(NT + 1):
            if i < NT:
                stF1(i)
            if 0 <= i - 1 < NT:
                stF2(i - 1)
            if i < NT:
                stF1b(i)
    finish(b, out_d)
    return b, None


def finish(b, out_d):
    b.barrier()
    return None


_CACHE = {}


def _consts(half):
    ident = np.eye(P, dtype=np.float32)
    tril = np.tril(np.ones((P, P), np.float32))
    ustrict = np.triu(np.ones((P, P), np.float32), 1)
    j = np.arange(P)[:, None]
    i = np.arange(P)[None, :]
    cur = np.where(j <= i, 0.0, NEG).astype(np.float32)
    prev = np.where(j >= i, 0.0, NEG).astype(np.float32)
    prev_halo = prev if half == 1 else np.full((P, P), NEG, np.float32)
    maskb = np.stack([cur, prev, prev_halo], axis=1)
    ecap = (np.arange(NE, dtype=np.float32) * CAP)[None, :]
    return dict(ident=ident, tril=tril, ustrict=ustrict, maskb=np.ascontiguousarray(maskb), ecap=ecap)


def make_in_maps(x, c, w_mod, b_mod, g_pre_mix, g_post_mix, w_in, g_gmlp_v, w_spatial,
                 b_spatial, g_out_gmlp, g_out_attn, w_out, g_pre_ffn, g_post_ffn,
                 w_router_group, b_router_group, w_router_expert, b_router_expert,
                 w_gate, w_up, w_down):
    f = lambda a: np.ascontiguousarray(np.asarray(a, dtype=np.float32))
    shared = dict(
        w_mod=f(w_mod[0]), b_mod=f(b_mod[0][None, :]),
        g_pre_mix=f(g_pre_mix[0][None, :]), g_post_mix=f(g_post_mix[0][None, :]),
        g_pre_ffn=f(g_pre_ffn[0][None, :]), g_post_ffn=f(g_post_ffn[0][None, :]),
        g_gmlp_v=f(g_gmlp_v[0][None, :]),
        g_out=f(np.concatenate([g_out_gmlp[0], g_out_attn[0]]).reshape(KC, P).T),
        w_in=f(w_in[0]), w_out=f(w_out[0]),
        w_sp=f(w_spatial[0]), b_sp=f(b_spatial[0].reshape(1, NH * P)),
        w_r=f(np.concatenate([w_router_group[0], np.transpose(w_router_expert[0], (1, 0, 2)).reshape(D, 32)], axis=1)),
        b_r=f(np.concatenate([b_router_group[0], b_router_expert[0].reshape(32)])[None, :]),
        w_gate=f(w_gate[0]), w_up=f(w_up[0]), w_down=f(w_down[0]),
    )
    maps = []
    x = np.asarray(x, dtype=np.float32)
    for core in range(8):
        bi, half = core // 2, core % 2
        m = dict(shared)
        m["x"] = f(x[bi, half * TOWN:(half + 1) * TOWN])
        m["xh"] = f(x[bi, 2048:4096]) if half == 1 else f(x[bi, 0:2048])
        m["c"] = f(np.asarray(c[bi], dtype=np.float32).reshape(KC, P).T)
        m.update(_consts(half))
        maps.append(m)
    return maps


def kernel(**inputs):
    if "nc" not in _CACHE:
        b, _ = build()
        _CACHE["nc"] = b.nc
    nc = _CACHE["nc"]
    maps = make_in_maps(**inputs)
    res = run_bass_kernel_spmd(nc, maps, core_ids=list(range(8)))
    out = np.empty((4, 8192, D), np.float32)
    for core in range(8):
        bi, half = core // 2, core % 2
        out[bi, half * TOWN:(half + 1) * TOWN] = res.results[core]["out"]
    if DEBUG:
        _CACHE["res"] = res
    return out
```
